# Optimizing a Trainium2 kernel written in Bass

```python
import math
import jax, jax.numpy as jnp
from jax import lax
import numpy as np

D_MODEL = 1024
BATCH = 2
SEQ = 16384
DEPTH = 2

LRU_W = 256
LRU_BLOCKS = 4
LRU_BLOCK_W = LRU_W // LRU_BLOCKS
LRU_C = 8.0
CONV_W = 4
MLSTM_HEADS = 4
MLSTM_HEAD_DIM = 64
MLSTM_W = MLSTM_HEADS * MLSTM_HEAD_DIM
MLSTM_CHUNK = 64
SB_HEADS = 4
SB_HEAD_DIM = 64
SB_W = SB_HEADS * SB_HEAD_DIM
SB_BLOCK = 128
FF = ((8 * D_MODEL // 3 + 255) // 256) * 256
EPS = 1e-6

SPLIT_SIZES = (LRU_W, LRU_W, 2 * MLSTM_W, MLSTM_W, MLSTM_W, MLSTM_HEADS, MLSTM_HEADS,
               SB_W, SB_W, SB_W, D_MODEL, D_MODEL, D_MODEL)
IN_COLS = sum(SPLIT_SIZES)

kernel_name = "hybrid_rglru_mlstm_stickbreaking_block"


def rmsnorm(x, g):
    xf = x.astype(jnp.float32)
    y = xf * lax.rsqrt(jnp.mean(xf * xf, axis=-1, keepdims=True) + EPS)
    return (y * g.astype(jnp.float32)).astype(x.dtype)


def causal_dwconv(x, w):
    K, C = w.shape
    return lax.conv_general_dilated(
        x, w.astype(x.dtype)[:, None, :], window_strides=(1,), padding=[(K - 1, 0)],
        dimension_numbers=("NWC", "WIO", "NWC"), feature_group_count=C)


def to_heads(t, n_heads):
    B, S, W = t.shape
    return t.reshape(B, S, n_heads, W // n_heads).transpose(0, 2, 1, 3)


def from_heads(t):
    B, H, S, d = t.shape
    return t.transpose(0, 2, 1, 3).reshape(B, S, H * d)


def rg_lru(xb, wa, ba, wx, bx, lam):
    B, S, W = xb.shape
    xf = xb.astype(jnp.float32)
    xh = xf.reshape(B, S, LRU_BLOCKS, LRU_BLOCK_W)
    r = jax.nn.sigmoid(jnp.einsum('bsgi,gij->bsgj', xh, wa.astype(jnp.float32)).reshape(B, S, W)
                       + ba.astype(jnp.float32))
    i = jax.nn.sigmoid(jnp.einsum('bsgi,gij->bsgj', xh, wx.astype(jnp.float32)).reshape(B, S, W)
                       + bx.astype(jnp.float32))
    log_a = -LRU_C * r * jax.nn.softplus(-lam.astype(jnp.float32))
    a = jnp.exp(log_a)
    u = jnp.sqrt(-jnp.expm1(2.0 * log_a)) * (i * xf)

    def combine(left, right):
        a1, b1 = left
        a2, b2 = right
        return a1 * a2, a2 * b1 + b2

    _, h = lax.associative_scan(combine, (a, u), axis=1)
    return h.astype(xb.dtype)


def mlstm_chunkwise(q, k, v, i_pre, f_pre):
    B, H, S, dk = q.shape
    dv = v.shape[-1]
    L = MLSTM_CHUNK
    nc = S // L
    out_dtype = v.dtype
    q = q.astype(jnp.float32)
    k = k.astype(jnp.float32) * (dk ** -0.5)
    v = v.astype(jnp.float32)
    logi = i_pre.astype(jnp.float32)
    logf = jax.nn.log_sigmoid(f_pre.astype(jnp.float32))

    def chunks(t):
        return jnp.moveaxis(t.reshape((B, H, nc, L) + t.shape[3:]), 2, 0)

    tril = jnp.tril(jnp.ones((L, L), dtype=bool))

    def step(carry, inp):
        C, n, m = carry
        qc, kc, vc, ic, fc = inp
        b = jnp.cumsum(fc, axis=-1)
        D = jnp.where(tril, b[..., :, None] - b[..., None, :] + ic[..., None, :], -jnp.inf)
        m_inter = b + m[..., None]
        m_t = jnp.maximum(jnp.max(D, axis=-1), m_inter)
        P = jnp.exp(D - m_t[..., None]) * jnp.einsum('bhtk,bhsk->bhts', qc, kc)
        decay = jnp.exp(m_inter - m_t)
        num = (jnp.einsum('bhts,bhsv->bhtv', P, vc)
               + decay[..., None] * jnp.einsum('bhvk,bhtk->bhtv', C, qc))
        den = jnp.sum(P, axis=-1) + decay * jnp.einsum('bhk,bhtk->bht', n, qc)
        h = num / jnp.maximum(jnp.abs(den), jnp.exp(-m_t))[..., None]
        bL = b[..., -1]
        g = bL[..., None] - b + ic
        m_new = jnp.maximum(bL + m, jnp.max(g, axis=-1))
        wts = jnp.exp(g - m_new[..., None])
        carry_decay = jnp.exp(bL + m - m_new)
        C = carry_decay[..., None, None] * C + jnp.einsum('bhs,bhsv,bhsk->bhvk', wts, vc, kc)
        n = carry_decay[..., None] * n + jnp.einsum('bhs,bhsk->bhk', wts, kc)
        return (C, n, m_new), h

    init = (jnp.zeros((B, H, dv, dk), jnp.float32), jnp.zeros((B, H, dk), jnp.float32),
            jnp.zeros((B, H), jnp.float32))
    _, hs = lax.scan(step, init, (chunks(q), chunks(k), chunks(v), chunks(logi), chunks(logf)))
    return jnp.moveaxis(hs, 0, 2).reshape(B, H, S, dv).astype(out_dtype)


def stick_breaking_attention(q, k, v):
    B, H, S, d = q.shape
    nb = S // SB_BLOCK
    scale = 1.0 / math.sqrt(d)
    loc = jnp.arange(SB_BLOCK)
    diag_mask = loc[None, :] < loc[:, None]
    outs = []
    for bi in range(nb):
        nk = bi + 1
        lk_len = nk * SB_BLOCK
        qblk = q[:, :, bi * SB_BLOCK:(bi + 1) * SB_BLOCK]
        kk = k[:, :, :lk_len]
        vv = v[:, :, :lk_len]
        z = jnp.einsum('bhqd,bhsd->bhqs', qblk, kk).astype(jnp.float32) * scale
        causal = jnp.concatenate(
            [jnp.ones((SB_BLOCK, lk_len - SB_BLOCK), dtype=bool), diag_mask], axis=1)
        log_keep = jnp.where(causal, jax.nn.log_sigmoid(-z), 0.0)
        intra = lax.cumsum(log_keep.reshape(B, H, SB_BLOCK, nk, SB_BLOCK), axis=4, reverse=True)
        bsum = intra[..., 0]
        inter = lax.cumsum(bsum, axis=3, reverse=True) - bsum
        R = (intra + inter[..., None]).reshape(B, H, SB_BLOCK, lk_len)
        A = jnp.where(causal, jnp.exp(z + R), 0.0)
        outs.append(jnp.einsum('bhqs,bhsd->bhqd', A.astype(v.dtype), vv))
    return jnp.concatenate(outs, axis=2)


def setup_inputs(seed: int = 0) -> dict:
    key = jax.random.key(seed)
    ks = jax.random.split(key, 24)
    f32 = jnp.float32
    D = D_MODEL

    def nrm(k, shape, fan_in):
        return jax.random.normal(k, shape, f32) * (fan_in ** -0.5)

    a0 = jax.random.uniform(ks[8], (DEPTH, LRU_W), f32, 0.9, 0.999)
    s0 = a0 ** (1.0 / LRU_C)
    lru_lambda = jnp.log(s0) - jnp.log1p(-s0)
    fg_b = (jnp.linspace(3.0, 6.0, MLSTM_HEADS, dtype=f32)[None, :]
            + 0.1 * jax.random.normal(ks[11], (DEPTH, MLSTM_HEADS), f32))
    return {
        "x": jax.random.normal(ks[0], (BATCH, SEQ, D), f32),
        "norm_mix_g": 1.0 + 0.02 * jax.random.normal(ks[1], (DEPTH, D), f32),
        "w_in": nrm(ks[2], (DEPTH, D, IN_COLS), D),
        "conv_lru_w": nrm(ks[3], (DEPTH, CONV_W, LRU_W), CONV_W),
        "lru_wa": nrm(ks[4], (DEPTH, LRU_BLOCKS, LRU_BLOCK_W, LRU_BLOCK_W), LRU_BLOCK_W),
        "lru_ba": 0.02 * jax.random.normal(ks[5], (DEPTH, LRU_W), f32),
        "lru_wx": nrm(ks[6], (DEPTH, LRU_BLOCKS, LRU_BLOCK_W, LRU_BLOCK_W), LRU_BLOCK_W),
        "lru_bx": 0.02 * jax.random.normal(ks[7], (DEPTH, LRU_W), f32),
        "lru_lambda": lru_lambda,
        "conv_mlstm_w": nrm(ks[9], (DEPTH, CONV_W, 2 * MLSTM_W), CONV_W),
        "mlstm_ig_b": 0.1 * jax.random.normal(ks[10], (DEPTH, MLSTM_HEADS), f32),
        "mlstm_fg_b": fg_b,
        "w_out_lru": nrm(ks[12], (DEPTH, LRU_W, D), LRU_W),
        "w_out_mlstm": nrm(ks[13], (DEPTH, MLSTM_W, D), MLSTM_W),
        "w_out_sb": nrm(ks[14], (DEPTH, SB_W, D), SB_W),
        "w_o": nrm(ks[15], (DEPTH, D, D), D),
        "norm_ffn_g": 1.0 + 0.02 * jax.random.normal(ks[16], (DEPTH, D), f32),
        "w_ffn_in": nrm(ks[17], (DEPTH, D, 2 * FF), D),
        "w_ffn_out": nrm(ks[18], (DEPTH, FF, D), FF),
        "final_norm_g": 1.0 + 0.02 * jax.random.normal(ks[19], (D,), f32),
    }


def reference(x, norm_mix_g, w_in, conv_lru_w, lru_wa, lru_ba, lru_wx, lru_bx, lru_lambda,
              conv_mlstm_w, mlstm_ig_b, mlstm_fg_b, w_out_lru, w_out_mlstm, w_out_sb, w_o,
              norm_ffn_g, w_ffn_in, w_ffn_out, final_norm_g):
    split_points = np.cumsum(SPLIT_SIZES)[:-1].tolist()
    for l in range(DEPTH):
        h = rmsnorm(x, norm_mix_g[l])
        proj = h @ w_in[l]
        (lru_x, lru_gate, m_qk, m_v, m_o, m_i, m_f,
         s_q, s_k, s_v, g_a, g_b, g_c) = jnp.split(proj, split_points, axis=-1)

        xa = causal_dwconv(lru_x, conv_lru_w[l])
        ya = rg_lru(xa, lru_wa[l], lru_ba[l], lru_wx[l], lru_bx[l], lru_lambda[l])
        ya = (ya * jax.nn.gelu(lru_gate)) @ w_out_lru[l]

        qk = jax.nn.silu(causal_dwconv(m_qk, conv_mlstm_w[l]))
        mq, mk = jnp.split(qk, 2, axis=-1)
        i_pre = (m_i + mlstm_ig_b[l]).transpose(0, 2, 1)
        f_pre = (m_f + mlstm_fg_b[l]).transpose(0, 2, 1)
        hb = mlstm_chunkwise(to_heads(mq, MLSTM_HEADS), to_heads(mk, MLSTM_HEADS),
                             to_heads(m_v, MLSTM_HEADS), i_pre, f_pre)
        yb = (jax.nn.sigmoid(m_o) * from_heads(hb)) @ w_out_mlstm[l]

        hc = stick_breaking_attention(to_heads(s_q, SB_HEADS), to_heads(s_k, SB_HEADS),
                                      to_heads(s_v, SB_HEADS))
        yc = from_heads(hc) @ w_out_sb[l]

        merged = (jax.nn.sigmoid(g_a) * ya + jax.nn.sigmoid(g_b) * yb
                  + jax.nn.sigmoid(g_c) * yc)
        x = x + merged @ w_o[l]

        hf = rmsnorm(x, norm_ffn_g[l])
        gate, up = jnp.split(hf @ w_ffn_in[l], 2, axis=-1)
        x = x + (jax.nn.silu(gate) * up) @ w_ffn_out[l]
    return rmsnorm(x, final_norm_g)
```

```python
import math
import numpy as np
import ml_dtypes
import concourse.bass as bass
import concourse.mybir as mybir
from concourse.alu_op_type import AluOpType as ALU
from concourse.bass_utils import run_bass_kernel_spmd

F32 = mybir.dt.float32
BF16 = mybir.dt.bfloat16
AF = mybir.ActivationFunctionType

D = 1024
FF = 2816
NCORES = 8
EPS = 1e-6
GELU_C = math.sqrt(2.0 / math.pi)

PE, ACT, DVE, POOL, SP = "pe", "act", "dve", "pool", "sp"
ENGS = (PE, ACT, DVE, POOL, SP)
N_DMA_SEMS = 10
N_CC_SEMS = 4


class Buf:
    __slots__ = ("name", "w", "r")

    def __init__(self, name):
        self.name = name
        self.w = None
        self.r = {}


class V:
    __slots__ = ("ap", "buf")

    def __init__(self, ap, buf):
        self.ap = ap
        self.buf = buf

    def __getitem__(self, k):
        return V(self.ap[k], self.buf)

    def re(self, s, **kw):
        return V(self.ap.rearrange(s, **kw), self.buf)

    def bc(self, dt):
        return V(self.ap.bitcast(dt), self.buf)


class Prog:
    def __init__(self, nc, same_engine_sync=True):
        self.nc = nc
        self.q = {e: [] for e in ENGS}
        self.sems = {}
        self.cnt = {}
        self.seen = {e: {} for e in ENGS}
        self.same = same_engine_sync
        self.dma_rr = {e: 0 for e in ENGS}
        self.cc_rr = 0
        self.dma_last = {}
        self.epoch = 0
        self._ctx = []

    def _mk(self, key):
        cm = self.nc.semaphore("s_" + "_".join(str(k) for k in key))
        self.sems[key] = cm.__enter__()
        self._ctx.append(cm)
        self.cnt[key] = 0

    def close(self):
        for cm in reversed(self._ctx):
            cm.__exit__(None, None, None)

    def _ckey(self, eng):
        key = ("c", eng, self.epoch)
        if key not in self.sems:
            self._mk(key)
        return key

    def _need(self, eng, waits, key, val):
        if key[0] == "c":
            if key[2] < self.epoch:
                return
            if key[1] == eng and (eng == PE or not self.same):
                return
        if self.seen[eng].get(key, 0) >= val:
            return
        if waits.get(key, 0) < val:
            waits[key] = val

    def op(self, eng, fn, reads=(), writes=(), dma=False, inc=True):
        waits = {}
        for v in reads:
            b = v.buf if isinstance(v, V) else v
            if b is not None and b.w is not None:
                self._need(eng, waits, *b.w)
        for v in writes:
            b = v.buf if isinstance(v, V) else v
            if b is None:
                continue
            if b.w is not None:
                self._need(eng, waits, *b.w)
            for k, val in b.r.items():
                self._need(eng, waits, k, val)
        if dma == "cc":
            i = self.cc_rr
            self.cc_rr = (i + 1) % N_CC_SEMS
            key = ("cc", i)
            step = 1
        elif dma:
            i = self.dma_rr[eng]
            self.dma_rr[eng] = (i + 1) % N_DMA_SEMS
            key = ("d", eng, i)
            step = 16
        else:
            key = self._ckey(eng)
            step = 1
        if dma:
            if key not in self.sems:
                self._mk(key)
            if key in self.dma_last:
                self._need(eng, waits, *self.dma_last[key])
        for k, v in waits.items():
            self.seen[eng][k] = v
        if inc:
            self.cnt[key] += step
            tok = (key, self.cnt[key])
        else:
            tok = (key, self.cnt[key] + step)
        if dma:
            self.dma_last[key] = tok
        self.q[eng].append((list(waits.items()), fn, key, step if inc else 0))
        for v in reads:
            b = v.buf if isinstance(v, V) else v
            if b is not None:
                if b.r.get(key, 0) < tok[1]:
                    b.r[key] = tok[1]
        for v in writes:
            b = v.buf if isinstance(v, V) else v
            if b is not None:
                b.w = tok
                b.r = {}
        return tok

    def barrier(self):
        snap = {k: c for k, c in self.cnt.items() if c > 0 and not (k[0] == "c" and k[2] < self.epoch)}
        for e in ENGS:
            waits = {}
            for k, c in snap.items():
                self._need(e, waits, k, c)
            for k, v in waits.items():
                self.seen[e][k] = v
            if waits:
                self.q[e].append((list(waits.items()), None, None, 0))
        self.epoch += 1

    def emit(self):
        nc = self.nc
        with nc.Block() as block:
            def run(e):
                def body(eng):
                    for waits, fn, key, inc in self.q[e]:
                        for k, v in waits:
                            eng.wait_ge(self.sems[k], v)
                        if fn is not None:
                            ins = fn(eng)
                            if inc:
                                if key[0] == "cc":
                                    ins.then_inc(self.sems[key])
                                else:
                                    ins.then_inc(self.sems[key], inc)
                return body
            block.tensor(run(PE))
            block.scalar(run(ACT))
            block.vector(run(DVE))
            block.gpsimd(run(POOL))
            block.sync(run(SP))


def _ap(x):
    return x.ap if isinstance(x, V) else x


def _vs(*xs):
    return [x for x in xs if isinstance(x, V)]


class K:
    def __init__(self, nc, P, arena, banks):
        self.nc = nc
        self.P = P
        self.arena = arena
        self.off = 0
        self.banks = banks
        self.bank_rr = 0
        self.marks = []

    def alloc(self, name, shape, dt, parts=128):
        n = int(np.prod(shape))
        nb = n * (4 if dt == F32 else 2)
        nb = (nb + 31) // 32 * 32
        ap = self.arena[:, self.off // 2:(self.off + nb) // 2]
        self.off += nb
        assert self.off <= 206 * 1024, (name, self.off)
        if dt == F32:
            ap = ap.bitcast(F32)
        ap = ap[:, 0:n]
        if parts != 128:
            ap = ap[0:parts]
        if len(shape) == 2:
            ap = ap.rearrange("p (a b) -> p a b", a=shape[0])
        elif len(shape) == 3:
            ap = ap.rearrange("p (a b c) -> p a b c", a=shape[0], b=shape[1])
        return V(ap, Buf(name))

    def mark(self):
        self.marks.append(self.off)

    def release(self):
        self.off = self.marks.pop()

    def bank(self):
        b = self.banks[self.bank_rr]
        self.bank_rr = (self.bank_rr + 1) % len(self.banks)
        return b

    def mm(self, out, lhsT, rhs, start, stop, inc=None, sgc=False):
        o, l, r = _ap(out), _ap(lhsT), _ap(rhs)
        self.P.op(PE, lambda e: e.matmul(o, lhsT=l, rhs=r, start=start, stop=stop, skip_group_check=sgc),
                  reads=_vs(lhsT, rhs), writes=_vs(out), inc=(stop if inc is None else inc))

    def tr(self, out, in_, ident, inc=True):
        o, i, d = _ap(out), _ap(in_), _ap(ident)
        self.P.op(PE, lambda e: e.transpose(o, i, d), reads=_vs(in_, ident), writes=_vs(out), inc=inc)

    def allgather(self, out, in_, groups):
        o, i = _ap(out), _ap(in_)
        self.P.op(POOL, lambda e: e.collective_compute("AllGather", ALU.bypass, replica_groups=groups,
                                                       ins=[i], outs=[o]),
                  reads=_vs(in_), writes=_vs(out), dma="cc")

    def act(self, out, in_, func, bias=None, scale=None, accum=None):
        o, i = _ap(out), _ap(in_)
        kw = {}
        if bias is not None:
            kw["bias"] = _ap(bias)
        if scale is not None:
            kw["scale"] = _ap(scale)
        if accum is not None:
            kw["accum_out"] = _ap(accum)
        self.P.op(ACT, lambda e: e.activation(out=o, in_=i, func=func, **kw),
                  reads=_vs(in_, bias, scale), writes=_vs(out, accum))

    def tt(self, out, in0, in1, op, eng=DVE):
        o, a, b = _ap(out), _ap(in0), _ap(in1)
        self.P.op(eng, lambda e: e.tensor_tensor(out=o, in0=a, in1=b, op=op),
                  reads=_vs(in0, in1), writes=_vs(out))

    def ts(self, out, in0, s1, op0, s2=None, op1=None, eng=DVE):
        o, a, x1, x2 = _ap(out), _ap(in0), _ap(s1), _ap(s2)
        if op1 is None:
            fn = lambda e: e.tensor_scalar(out=o, in0=a, scalar1=x1, scalar2=None, op0=op0)
        else:
            fn = lambda e: e.tensor_scalar(out=o, in0=a, scalar1=x1, scalar2=x2, op0=op0, op1=op1)
        self.P.op(eng, fn, reads=_vs(in0, s1, s2), writes=_vs(out))

    def stt(self, out, in0, scalar, in1, op0, op1, eng=DVE):
        o, a, s, b = _ap(out), _ap(in0), _ap(scalar), _ap(in1)
        self.P.op(eng, lambda e: e.scalar_tensor_tensor(out=o, in0=a, scalar=s, in1=b, op0=op0, op1=op1),
                  reads=_vs(in0, scalar, in1), writes=_vs(out))

    def recip(self, out, in_):
        o, i = _ap(out), _ap(in_)
        self.P.op(DVE, lambda e: e.reciprocal(out=o, in_=i), reads=_vs(in_), writes=_vs(out))

    def copy(self, out, in_, eng=DVE):
        o, i = _ap(out), _ap(in_)
        self.P.op(eng, lambda e: e.tensor_copy(out=o, in_=i), reads=_vs(in_), writes=_vs(out))

    def memset(self, out, val, eng=DVE):
        o = _ap(out)
        self.P.op(eng, lambda e: e.memset(o, val), writes=_vs(out))

    def scan(self, out, d0, d1, initial, op0, op1):
        o, a, b, i = _ap(out), _ap(d0), _ap(d1), _ap(initial)
        self.P.op(DVE, lambda e: e.tensor_tensor_scan(out=o, data0=a, data1=b, initial=i, op0=op0, op1=op1),
                  reads=_vs(d0, d1, initial), writes=_vs(out))

    def dma(self, out, in_, eng=SP):
        o, i = _ap(out), _ap(in_)
        self.P.op(eng, lambda e: e.dma_start(out=o, in_=i), reads=_vs(in_), writes=_vs(out), dma=True)

    def sigmoid_act(self, out, in_, tmp1, tmp2, scale=1.0, bias=None):
        self.act(tmp1, in_, AF.Exp, scale=-scale, bias=bias)
        self.act(tmp2, tmp1, AF.Ln, bias=1.0)
        self.act(out, tmp2, AF.Exp, scale=-1.0)


def load_cast(k, dst, src_ap, ncols, step=2048):
    c = 0
    while c < ncols:
        n = min(step, ncols - c)
        k.dma(dst[:, c:c + n], src_ap[:, c:c + n], eng=POOL)
        c += n


def rms_rstd(k, x, junk, ss, lnv, rstd, eps_col):
    k.act(junk, x, AF.Square, accum=ss)
    k.act(lnv, ss, AF.Ln, scale=1.0 / D, bias=eps_col)
    k.act(rstd, lnv, AF.Exp, scale=-0.5)


def norm_to_hT(k, x, g_bc, dstT, cst, tmp):
    rms_rstd(k, x, tmp["junk"], tmp["ss"], tmp["lnv"], tmp["rstd"], cst["eps"])
    k.stt(tmp["hb"], x, tmp["rstd"], g_bc, ALU.mult, ALU.mult)
    bank = k.bank()
    pT = bank.bc(BF16)
    for c in range(8):
        k.tr(pT[:, c * 128:(c + 1) * 128], tmp["hb"][:, c * 128:(c + 1) * 128], cst["ident"], inc=(c == 7))
    k.act(dstT, pT.re("p (c t) -> p c t", c=8), AF.Copy)


class Builder:
    def __init__(self, name):
        self.nc = bass.Bass("TRN2", target_bir_lowering=False)
        self.stack = []
        nc = self.nc
        self.P = Prog(nc)
        cm = nc.sbuf_tensor("arena", [128, 103 * 1024], BF16)
        arena = cm.__enter__()
        self.stack.append(cm)
        cm = nc.psum_tensor("ps_all", [128, 4096], F32)
        ps_all = cm.__enter__()
        self.stack.append(cm)
        self.ps_all = ps_all
        banks = [V(ps_all[:, i * 512:(i + 1) * 512], Buf(f"ps{i}")) for i in range(8)]
        self.k = K(nc, self.P, arena[:], banks)
        self.k.ps_all = ps_all
        self.outs = []

    def inp(self, name, shape, dt=F32):
        return V(self.nc.dram_tensor(name, list(shape), dt, kind="ExternalInput").ap(), Buf(name))

    def out(self, name, shape, dt=F32):
        v = V(self.nc.dram_tensor(name, list(shape), dt, kind="ExternalOutput").ap(), Buf(name))
        self.outs.append(v)
        return v

    def scratch(self, name, shape, dt=F32):
        return V(self.nc.dram_tensor(name, list(shape), dt, kind="Internal").ap(), Buf(name))

    def dram(self, name, shape, dt=F32):
        return V(self.nc.dram_tensor(name, list(shape), dt).ap(), Buf(name))

    def finish(self):
        P = self.P
        waits = {}
        for v in self.outs:
            if v.buf.w is not None:
                P._need(SP, waits, *v.buf.w)
        P.q[SP].append((list(waits.items()), None, None, 0))
        P.emit()
        P.close()
        for cm in reversed(self.stack):
            cm.__exit__(None, None, None)
        return self.nc


def load_consts(bd, k, cdram):
    cst = {}
    cst["ident"] = k.alloc("ident", (128,), BF16)
    cst["eps"] = k.alloc("eps", (1,), F32)
    k.dma(cst["ident"], cdram[:, 0:128], eng=POOL)
    k.memset(cst["eps"], EPS)
    return cst


def const_table():
    t = np.zeros((128, 128), np.float32)
    t[:, 0:128] = np.eye(128, dtype=np.float32)
    return t


GROUPS = [[0, 1, 2, 3], [4, 5, 6, 7]]


def emit_pre(bd, k, cst, x_d, g_d, hT_loc, gatH, TS):
    k.mark()
    g_bc = k.alloc("g_bc", (1024,), F32)
    k.dma(g_bc, g_d)
    xs = [k.alloc(f"pre_x{i}", (1024,), F32) for i in range(2)]
    stg = [k.alloc(f"pre_stg{i}", (8, 512), BF16) for i in range(2)]
    tmp = dict(junk=V(k.alloc("pre_junk", (1024,), BF16).ap, None),
               ss=k.alloc("pre_ss", (1,), F32), lnv=k.alloc("pre_lnv", (1,), F32),
               rstd=k.alloc("pre_rstd", (1,), F32), hb=k.alloc("pre_hb", (1024,), BF16))
    nt = TS // 128
    k.dma(xs[0], x_d[0:128, :])
    for i in range(nt):
        if i + 1 < nt:
            k.dma(xs[(i + 1) % 2], x_d[(i + 1) * 128:(i + 2) * 128, :])
        sg = stg[(i // 4) % 2]
        norm_to_hT(k, xs[i % 2], g_bc, sg[:, :, (i % 4) * 128:(i % 4 + 1) * 128], cst, tmp)
        if i % 4 == 3:
            c = i // 4
            k.dma(hT_loc[c].re("(c p) t -> p c t", p=128), sg)
            k.allgather(gatH[c], hT_loc[c], GROUPS)
    k.release()
    k.P.barrier()


def emit_tail(bd, k, cst, TS, x_d, hT_loc, YT_src, wg_d, wout_d, wo_d, g2_d, wfi_d, wfo_d, gn_d,
              xmid_d, xout_d, hTn_loc, gatHn, last):
    P = k.P
    k.mark()
    wg = [[k.alloc(f"wg{i}_{p}", (1536,), BF16) for p in range(2)] for i in range(8)]
    wout = [k.alloc(f"wout{i}", (1024,), BF16) for i in range(6)]
    wo = [k.alloc(f"wo{i}", (1024,), BF16) for i in range(8)]
    for kc in range(8):
        k.dma(wg[kc][0], wg_d[kc * 128:(kc + 1) * 128, 0:1536], eng=POOL)
    for kc in range(6):
        load_cast(k, wout[kc], wout_d[kc * 128:(kc + 1) * 128, :], 1024)
    for kc in range(8):
        k.dma(wg[kc][1], wg_d[kc * 128:(kc + 1) * 128, 1536:3072], eng=POOL)
    for kc in range(8):
        load_cast(k, wo[kc], wo_d[kc * 128:(kc + 1) * 128, :], 1024)
    hTb = [k.alloc(f"a_hT{i}", (8, 512), BF16) for i in range(2)]
    YTb = [k.alloc(f"a_YT{i}", (6, 512), BF16) for i in range(2)]
    xb = [k.alloc(f"a_x{i}", (4, 1024), F32) for i in range(2)]
    mT = k.alloc("a_mT", (8, 512), BF16)
    e1 = [k.alloc(f"a_e1{i}", (512,), F32) for i in range(2)]
    e2 = [k.alloc(f"a_e2{i}", (512,), F32) for i in range(2)]
    sg = [k.alloc(f"a_sg{i}", (512,), F32) for i in range(2)]
    macc = k.alloc("a_macc", (512,), F32)
    mtmp = k.alloc("a_mtmp", (512,), F32)
    x_v = x_d.re("(i p) d -> p i d", p=128)
    xmid_v = xmid_d.re("(i p) d -> p i d", p=128)
    nblk = TS // 512

    def loadA(b):
        t0 = b * 512
        k.dma(hTb[b % 2], hT_loc[b].re("(c p) t -> p c t", p=128))
        for h in range(4):
            pb = (h % 2) * 64
            k.dma(YTb[b % 2][pb:pb + 64, (h // 2)::2, :], YT_src(b, h))
        k.dma(xb[b % 2], x_v[:, b * 4:(b + 1) * 4, :])

    loadA(0)
    cnt = 0
    for b in range(nblk):
        if b + 1 < nblk:
            loadA(b + 1)
        hT, YT, x = hTb[b % 2], YTb[b % 2], xb[b % 2]
        for oc in range(8):
            for br in range(3):
                G = k.bank()
                for kc in range(8):
                    c0 = br * 1024 + oc * 128
                    k.mm(G, wg[kc][c0 // 1536][:, c0 % 1536: c0 % 1536 + 128], hT[:, kc, :],
                         kc == 0, kc == 7)
                Y = k.bank()
                for kc in range(2):
                    k.mm(Y, wout[br * 2 + kc][:, oc * 128:(oc + 1) * 128], YT[:, br * 2 + kc, :],
                         kc == 0, kc == 1)
                j = cnt % 2
                cnt += 1
                k.sigmoid_act(sg[j], G, e1[j], e2[j])
                if br == 0:
                    k.tt(macc, Y, sg[j], ALU.mult)
                elif br == 1:
                    k.tt(mtmp, Y, sg[j], ALU.mult)
                    k.tt(macc, macc, mtmp, ALU.add, eng=POOL)
                else:
                    k.tt(mtmp, Y, sg[j], ALU.mult)
                    k.tt(mT[:, oc, :], macc, mtmp, ALU.add, eng=POOL)
        for i in range(4):
            for ch in range(2):
                O = k.bank()
                for kc in range(8):
                    k.mm(O, mT[:, kc, i * 128:(i + 1) * 128], wo[kc][:, ch * 512:(ch + 1) * 512],
                         kc == 0, kc == 7)
                k.tt(x[:, i, ch * 512:(ch + 1) * 512], x[:, i, ch * 512:(ch + 1) * 512], O, ALU.add)
        k.dma(V(xmid_v.ap[:, b * 4:(b + 1) * 4, :], Buf("xmid_blk")), x)
    k.release()
    P.barrier()

    k.mark()
    TB = 256
    NT = TB // 128
    wfi = [[k.alloc(f"wfi{i}_{p}", (1408,), BF16) for p in range(4)] for i in range(8)]
    wfo = [k.alloc(f"wfo{i}", (1024,), BF16) for i in range(22)]
    g2 = k.alloc("g2", (1024,), F32)
    gn = k.alloc("gn", (1024,), F32)
    k.dma(g2, g2_d)
    k.dma(gn, gn_d)
    for pc in (0, 2, 1, 3):
        for kc in range(8):
            k.dma(wfi[kc][pc], wfi_d[kc * 128:(kc + 1) * 128, pc * 1408:(pc + 1) * 1408], eng=POOL)
    for kc in range(22):
        load_cast(k, wfo[kc], wfo_d[kc * 128:(kc + 1) * 128, :], 1024)
    xm = [k.alloc(f"b_x{i}", (NT, 1024), F32) for i in range(2)]
    hfT = k.alloc("b_hfT", (8, TB), BF16)
    aT = k.alloc("b_aT", (22, TB), BF16)
    e1 = [k.alloc(f"b_e1{i}", (TB,), F32) for i in range(2)]
    e2 = [k.alloc(f"b_e2{i}", (TB,), F32) for i in range(2)]
    sg = [k.alloc(f"b_sg{i}", (TB,), F32) for i in range(2)]
    t1 = [k.alloc(f"b_t1{i}", (TB,), F32) for i in range(2)]
    tmp = dict(junk=V(k.alloc("b_junk", (1024,), BF16).ap, None),
               ss=k.alloc("b_ss", (1,), F32), lnv=k.alloc("b_lnv", (1,), F32),
               rstd=k.alloc("b_rstd", (1,), F32), hb=k.alloc("b_hb", (1024,), BF16))
    stg = k.alloc("b_stg", (8, TB), BF16)
    of = k.alloc("b_of", (NT, 1024), F32)
    nblk = TS // TB
    xout_v = xout_d.re("(i p) d -> p i d", p=128)

    def loadB(b):
        k.dma(xm[b % 2], xmid_v[:, b * NT:(b + 1) * NT, :])

    loadB(0)
    cnt = 0
    for b in range(nblk):
        if b + 1 < nblk:
            loadB(b + 1)
        x = xm[b % 2]
        for i in range(NT):
            norm_to_hT(k, x[:, i, :], g2, hfT[:, :, i * 128:(i + 1) * 128], cst, tmp)
        for hc in range(22):
            Gt = k.bank()
            Up = k.bank()
            for kc in range(8):
                k.mm(Gt[:, 0:TB], wfi[kc][(hc * 128) // 1408][:, (hc * 128) % 1408:(hc * 128) % 1408 + 128],
                     hfT[:, kc, :], kc == 0, kc == 7)
            for kc in range(8):
                u0 = FF + hc * 128
                k.mm(Up[:, 0:TB], wfi[kc][u0 // 1408][:, u0 % 1408:u0 % 1408 + 128], hfT[:, kc, :],
                     kc == 0, kc == 7)
            j = cnt % 2
            cnt += 1
            k.sigmoid_act(sg[j], Gt[:, 0:TB], e1[j], e2[j])
            k.tt(t1[j], Gt[:, 0:TB], sg[j], ALU.mult)
            k.tt(aT[:, hc, :], t1[j], Up[:, 0:TB], ALU.mult)
        for i in range(NT):
            for ch in range(2):
                O = k.bank()
                for hc in range(22):
                    k.mm(O, aT[:, hc, i * 128:(i + 1) * 128], wfo[hc][:, ch * 512:(ch + 1) * 512],
                         hc == 0, hc == 21)
                k.tt(x[:, i, ch * 512:(ch + 1) * 512], x[:, i, ch * 512:(ch + 1) * 512], O, ALU.add)
        if not last:
            k.dma(V(xout_v.ap[:, b * NT:(b + 1) * NT, :], Buf("xout_blk")), x)
            for i in range(NT):
                norm_to_hT(k, x[:, i, :], gn, stg[:, :, i * 128:(i + 1) * 128], cst, tmp)
            c = (b * TB) // 512
            k.dma(hTn_loc[c].re("(c p) t -> p c t", p=128)[:, :, (b * TB) % 512:(b * TB) % 512 + TB], stg)
            if (b * TB) % 512 + TB == 512:
                k.allgather(gatHn[c], hTn_loc[c], GROUPS)
        else:
            for i in range(NT):
                rms_rstd(k, x[:, i, :], tmp["junk"], tmp["ss"], tmp["lnv"], tmp["rstd"], cst["eps"])
                k.stt(of[:, i, :], x[:, i, :], tmp["rstd"], gn, ALU.mult, ALU.mult)
            k.dma(V(xout_v.ap[:, b * NT:(b + 1) * NT, :], Buf("xout_blk")), of)
    k.release()
    P.barrier()


C_LX, C_LG, C_MQ, C_MK, C_MO, C_F, C_I, C_SQ, C_SK, C_MV, C_SV, WH_COLS = \
    0, 64, 128, 192, 256, 320, 384, 448, 576, 704, 768, 896
CT_ID, CT_NTRI, CT_MM, CT_RST, CT_ATT, CT_COLS = 0, 128, 256, 384, 896, 2944


def const_table():
    t = np.zeros((128, CT_COLS), np.float32)
    t[:, CT_ID:CT_ID + 128] = np.eye(128, dtype=np.float32)
    j = np.arange(128)[:, None]
    s = np.arange(128)[None, :]
    t[:, CT_NTRI:CT_NTRI + 128] = -(j >= s).astype(np.float32)
    t[:, CT_MM:CT_MM + 128] = (j <= s).astype(np.float32)
    r = np.ones(512, np.float32)
    r[::128] = 0.0
    t[:, CT_RST:CT_RST + 512] = r[None, :]
    tq = np.arange(512)[None, :]
    for kj in range(4):
        t[:, CT_ATT + kj * 512: CT_ATT + (kj + 1) * 512] = ((kj * 128 + j) < tq).astype(np.float32)
    return t


def load_consts(bd, k, cdram, full=False):
    cst = {}
    n = CT_COLS if full else 128
    cb = k.alloc("cb", (n,), BF16)
    load_cast(k, cb, cdram, n)
    cst["ident"] = cb[:, CT_ID:CT_ID + 128]
    if full:
        cst["ntri"] = cb[:, CT_NTRI:CT_NTRI + 128]
        cst["mmask"] = cb[:, CT_MM:CT_MM + 128]
        cst["rst"] = cb[0:64, CT_RST:CT_RST + 512]
        cst["att"] = cb[:, CT_ATT:CT_ATT + 2048].re("p (j t) -> p j t", j=4)
        cst["nones"] = k.alloc("nones", (128,), BF16)
        k.memset(cst["nones"], -1.0)
    cst["eps"] = k.alloc("eps", (1,), F32)
    k.memset(cst["eps"], EPS)
    return cst


def conv4(k, out, xin, cw):
    k.ts(out, xin[:, 0:512], cw[:, 0:1], ALU.mult)
    for j in range(1, 4):
        k.stt(out, xin[:, j:j + 512], cw[:, j:j + 1], out, ALU.mult, ALU.add)
    k.copy(xin[:, 0:3], xin[:, 512:515])


def emit_mix(bd, k, cst, S, hT_src, wh_d, pm_d, wl_d, YT_loc, gatY, CW):
    def YT_dst(r0, t0):
        return YT_loc[t0 // CW][r0:r0 + 64, t0 % CW:t0 % CW + 512]

    P = k.P
    k.mark()
    NB = S // 512
    allb = k.banks
    k.banks = allb[0:6]
    k.bank_rr = 0
    bnum, bden = allb[6], allb[7]

    qT = k.alloc("qT", (S,), BF16)
    kT = k.alloc("kT", (S,), BF16)
    vsb = k.alloc("vsb", (S // 128, 128), BF16)
    W = [k.alloc(f"W{i}", (WH_COLS,), BF16) for i in range(8)]
    for kc in range(8):
        load_cast(k, W[kc], wh_d[kc * 128:(kc + 1) * 128, :], WH_COLS)
    pm = k.alloc("pm", (24,), F32, parts=64)
    k.dma(pm[:, 0:17], pm_d)
    wl = k.alloc("wl", (128,), BF16, parts=64)
    load_cast(k, wl, wl_d, 128)
    k.mark()

    def T(name, n=512, dt=F32, parts=64):
        return k.alloc(name, (n,), dt, parts)

    nba, nbx, nfb, ibm, nspl8 = pm[:, 17:18], pm[:, 18:19], pm[:, 19:20], pm[:, 20:21], pm[:, 21:22]
    k.ts(nba, pm[:, 12:13], -1.0, ALU.mult)
    k.ts(nbx, pm[:, 13:14], -1.0, ALU.mult)
    k.ts(nfb, pm[:, 16:17], -1.0, ALU.mult)
    k.ts(ibm, pm[:, 15:16], -math.log(8.0), ALU.add)
    k.act(pm[:, 22:23], pm[:, 14:15], AF.Exp, scale=-1.0)
    k.act(pm[:, 23:24], pm[:, 22:23], AF.Ln, bias=1.0)
    k.ts(nspl8, pm[:, 23:24], -8.0, ALU.mult)

    hTb = [k.alloc(f"m_hT{i}", (8, 512), BF16) for i in range(2)]
    xl, xq, xk = T("xl", 515), T("xq", 515), T("xk", 515)
    for t in (xl, xq, xk):
        k.memset(t[:, 0:3], 0.0)
    gen = [T(f"gen{i}") for i in range(16)]
    xabf = T("xabf", dt=BF16)
    tA, tB = T("tA"), T("tB")
    xa, gx, r_, ig, a_, a2, m1, u_, x2, p_, inner, sgl, yat = gen[0:13]
    cq, ck, ef, lf, nb, eb, itmp, ek, sq_, sk_, q1, k1, so, dd, rd, hm = gen[0:16]
    hl = [T("hl0"), T("hl1")]
    ya_o = [T("ya_o0", dt=BF16), T("ya_o1", dt=BF16)]
    yb_o = [T("yb_o0", dt=BF16), T("yb_o1", dt=BF16)]
    qtl, ktl = T("qtl", dt=BF16), T("ktl", dt=BF16)
    vmb = [k.alloc(f"vmb{i}", (4, 128), BF16) for i in range(2)]
    for v in vmb:
        k.memset(v[:, :, 64:128], 1.0)
    ktok = [k.alloc(f"ktok{i}", (64,), BF16) for i in range(2)]
    PTs = [k.alloc(f"PTs{i}", (128,), BF16) for i in range(2)]
    Wc = [T("Wc0", 128), T("Wc1", 128)]
    Dst = [T("Dst0", 128), T("Dst1", 128)]
    Dbf = [T("Dbf0", 128, BF16), T("Dbf1", 128, BF16)]
    ident = cst["ident"]

    def loadH(b):
        k.dma(hTb[b % 2], hT_src(b * 512))

    loadH(0)
    for b in range(NB):
        if b + 1 < NB:
            loadH(b + 1)
        hT = hTb[b % 2]
        t0 = b * 512
        tsl = slice(t0, t0 + 512)

        def proj(c0, M):
            bk = k.bank()
            for kc in range(8):
                k.mm(bk[0:M, :], W[kc][:, c0:c0 + M], hT[:, kc, :], kc == 0, kc == 7)
            return bk[0:M, :]

        k.act(qT[:, tsl], proj(C_SQ, 128), AF.Copy, scale=0.125)
        k.copy(kT[:, tsl], proj(C_SK, 128))
        bv = k.bank()
        for i in range(4):
            for kc in range(8):
                k.mm(bv[:, i * 128:(i + 1) * 128], hT[:, kc, i * 128:(i + 1) * 128],
                     W[kc][:, C_SV:C_SV + 128], kc == 0, kc == 7)
        k.act(vsb[:, 4 * b:4 * b + 4, :], bv.re("p (i c) -> p i c", i=4), AF.Copy)

        k.act(xl[:, 3:515], proj(C_LX, 64), AF.Copy)
        conv4(k, xa, xl, pm[:, 0:4])
        k.act(xabf, xa, AF.Copy)
        k.act(gx, proj(C_LG, 64), AF.Copy)
        br_ = k.bank()
        k.mm(br_[0:64, :], wl[:, 0:64], xabf, True, True)
        k.sigmoid_act(r_, br_[0:64, :], tA, tB, bias=nba)
        bi_ = k.bank()
        k.mm(bi_[0:64, :], wl[:, 64:128], xabf, True, True)
        k.sigmoid_act(ig, bi_[0:64, :], tA, tB, bias=nbx)
        k.act(a_, r_, AF.Exp, scale=nspl8)
        k.act(a2, a_, AF.Square)
        k.ts(a2, a2, 0.99999994, ALU.min)
        k.act(m1, a2, AF.Ln, scale=-1.0, bias=1.0)
        k.act(m1, m1, AF.Exp, scale=0.5)
        k.tt(u_, ig, xa, ALU.mult)
        k.tt(u_, u_, m1, ALU.mult)
        init = 0.0 if b == 0 else hl[(b - 1) % 2][:, 511:512]
        k.scan(hl[b % 2], a_, u_, init, ALU.mult, ALU.add)
        k.act(x2, gx, AF.Square)
        k.ts(p_, x2, 0.044715, ALU.mult, 1.0, ALU.add)
        k.tt(inner, p_, gx, ALU.mult)
        k.sigmoid_act(sgl, inner, tA, tB, scale=2.0 * GELU_C)
        k.tt(yat, hl[b % 2], gx, ALU.mult)
        k.tt(ya_o[b % 2], yat, sgl, ALU.mult)
        k.dma(YT_dst(0, t0), ya_o[b % 2])

        k.act(xq[:, 3:515], proj(C_MQ, 64), AF.Copy)
        k.act(xk[:, 3:515], proj(C_MK, 64), AF.Copy)
        conv4(k, cq, xq, pm[:, 4:8])
        conv4(k, ck, xk, pm[:, 8:12])
        k.act(ef, proj(C_F, 64), AF.Exp, scale=-1.0, bias=nfb)
        k.act(lf, ef, AF.Ln, bias=1.0)
        k.scan(nb, cst["rst"], lf, 0.0, ALU.mult, ALU.add)
        k.act(eb, nb, AF.Exp, scale=-1.0)
        k.tt(itmp, proj(C_I, 64), nb, ALU.add)
        k.act(ek, itmp, AF.Exp, bias=ibm)
        k.sigmoid_act(sq_, cq, tA, tB)
        k.tt(q1, cq, sq_, ALU.mult)
        k.tt(qtl, q1, eb, ALU.mult)
        k.sigmoid_act(sk_, ck, tA, tB)
        k.tt(k1, ck, sk_, ALU.mult)
        k.tt(ktl, k1, ek, ALU.mult)
        bmv = k.bank()
        for i in range(4):
            for kc in range(8):
                k.mm(bmv[:, i * 64:(i + 1) * 64], hT[:, kc, i * 128:(i + 1) * 128],
                     W[kc][:, C_MV:C_MV + 64], kc == 0, kc == 7)
        vm = vmb[b % 2]
        k.act(vm[:, :, 0:64], bmv[:, 0:256].re("p (i c) -> p i c", i=4), AF.Copy)
        k.sigmoid_act(so, proj(C_MO, 64), tA, tB)
        for c in range(4):
            cs = slice(c * 128, (c + 1) * 128)
            g = 4 * b + c
            bt = k.bank().bc(BF16)
            k.tr(bt[:, 0:64], ktl[:, cs], ident[0:64, 0:64])
            k.copy(ktok[g % 2], bt[:, 0:64])
            bp = k.bank()
            k.mm(bp[:, 0:128], ktl[:, cs], qtl[:, cs], True, True)
            k.tt(PTs[g % 2], bp[:, 0:128], cst["mmask"], ALU.mult)
            Dc = Dbf[g % 2]
            k.mm(bnum[0:64, cs], vm[:, c, 0:64], PTs[g % 2], True, g == 0)
            if g > 0:
                k.mm(bnum[0:64, cs], Dc[:, 0:64], qtl[:, cs], False, True)
            k.mm(bden[0:64, cs], vm[:, c, 64:128], PTs[g % 2], True, g == 0)
            if g > 0:
                k.mm(bden[0:64, cs], Dc[:, 64:128], qtl[:, cs], False, True)
            bu = k.bank()
            k.mm(bu[0:64, 0:128], ktok[g % 2], vm[:, c, :], True, True)
            a_c = eb[:, c * 128 + 127:c * 128 + 128]
            k.act(Wc[g % 2], bu[0:64, 0:128], AF.Identity, scale=a_c)
            if g == 0:
                k.copy(Dst[1], Wc[0])
            else:
                k.stt(Dst[(g + 1) % 2], Dst[g % 2], a_c, Wc[g % 2], ALU.mult, ALU.add)
            k.act(Dbf[(g + 1) % 2], Dst[(g + 1) % 2], AF.Copy)
        k.act(dd, bden[0:64, :], AF.Abs)
        k.ts(dd, dd, 1.0, ALU.max)
        k.recip(rd, dd)
        k.tt(hm, bnum[0:64, :], rd, ALU.mult)
        k.tt(yb_o[b % 2], hm, so, ALU.mult)
        k.dma(YT_dst(64, t0), yb_o[b % 2])
    k.release()
    P.barrier()

    k.mark()
    zp = [V(k.ps_all[:, i * 1024:(i + 1) * 1024], Buf(f"zp{i}")) for i in range(3)]
    ob = allb[6:8]
    e_t = [k.alloc(f"s2e{i}", (1024,), F32) for i in range(2)]
    sp = [k.alloc(f"s2sp{i}", (1024,), BF16) for i in range(3)]
    At = [k.alloc(f"s2A{i}", (1024,), BF16) for i in range(3)]
    accA = [k.alloc(f"s2accA{i}", (512,), BF16) for i in range(2)]
    accM = [k.alloc(f"s2accM{i}", (512,), BF16) for i in range(2)]
    yc_o = [k.alloc(f"s2yc{i}", (512,), BF16, parts=64) for i in range(2)]
    ntri, nones, att = cst["ntri"], cst["nones"], cst["att"]
    H0, H1 = slice(0, 512), slice(512, 1024)
    p_glob = 0
    for qi in range(S // 512):
        q0 = qi * 512
        O = ob[qi % 2]
        kbs = list(range(4 * qi + 3, -1, -1))
        NP = len(kbs) // 2
        qv = qT[:, q0:q0 + 512]

        def stageA(p):
            khi, klo = kbs[2 * p], kbs[2 * p + 1]
            Z = zp[(p_glob + p) % 3]
            k.mm(Z[:, H0], kT[:, khi * 128:(khi + 1) * 128], qv, True, True, inc=False)
            k.mm(Z[:, H1], kT[:, klo * 128:(klo + 1) * 128], qv, True, True, inc=True)
            k.act(e_t[p % 2], Z, AF.Exp)
            k.act(sp[p % 3], e_t[p % 2], AF.Ln, bias=1.0)
            if klo >= 4 * qi:
                k.tt(sp[p % 3][:, H0], sp[p % 3][:, H0], att[:, khi - 4 * qi, :], ALU.mult)
                k.tt(sp[p % 3][:, H1], sp[p % 3][:, H1], att[:, klo - 4 * qi, :], ALU.mult)

        def stageB1(p):
            khi, klo = kbs[2 * p], kbs[2 * p + 1]
            Z = zp[(p_glob + p) % 3]
            s_ = sp[p % 3]
            k.mm(Z[:, H0], ntri, s_[:, H0], False, p == 0, sgc=True)
            if p > 0:
                k.mm(Z[:, H0], nones, accA[p % 2], False, True, sgc=True)
                k.tt(accM[p % 2], accA[p % 2], s_[:, H0], ALU.add)
                mid = accM[p % 2]
            else:
                mid = s_[:, H0]
            k.mm(Z[:, H1], ntri, s_[:, H1], False, False, sgc=True)
            k.mm(Z[:, H1], nones, mid, False, True, sgc=True)
            k.act(At[p % 3], Z, AF.Exp)
            if klo >= 4 * qi:
                k.tt(At[p % 3][:, H0], At[p % 3][:, H0], att[:, khi - 4 * qi, :], ALU.mult)
                k.tt(At[p % 3][:, H1], At[p % 3][:, H1], att[:, klo - 4 * qi, :], ALU.mult)
            if p + 1 < NP:
                k.tt(accA[(p + 1) % 2], mid, s_[:, H1], ALU.add)

        def stageB2(p):
            khi, klo = kbs[2 * p], kbs[2 * p + 1]
            k.mm(O, vsb[:, khi, :], At[p % 3][:, H0], p == 0, False)
            k.mm(O, vsb[:, klo, :], At[p % 3][:, H1], False, p == NP - 1)

        for step in range(NP + 2):
            if step < NP:
                stageA(step)
            if 1 <= step <= NP:
                stageB1(step - 1)
            if step >= 2:
                stageB2(step - 2)
        p_glob += NP
        k.act(yc_o[qi % 2], O[0:64, :], AF.Copy)
        k.dma(YT_dst(128, q0), yc_o[qi % 2])
        if (q0 + 512) % CW == 0:
            c = q0 // CW
            k.allgather(gatY[c], YT_loc[c], GROUPS)
    k.release()
    k.release()
    k.banks = allb
    P.barrier()


def build_fused(S):
    TS = S // 4
    NCH = TS // 512
    CW = min(2048, TS)
    NYC = S // CW
    CPS = TS // CW
    bd = Builder("fused")
    k = bd.k
    nc = bd.nc
    x_d = bd.inp("x", [TS, D])
    c_d = bd.inp("cst", [128, CT_COLS])
    gm_d = [bd.inp("gmix0", [128, D]), bd.inp("gn0", [128, D])]
    gn_d = [gm_d[1], bd.inp("gn1", [128, D])]
    L = []
    for l in range(2):
        L.append(dict(wh=bd.inp(f"wh{l}", [D, WH_COLS]), pm=bd.inp(f"pm{l}", [64, 17]), wl=bd.inp(f"wl{l}", [64, 128]),
                      wg=bd.inp(f"wg{l}", [D, 3072]), wout=bd.inp(f"wout{l}", [768, D]), wo=bd.inp(f"wo{l}", [D, D]),
                      g2=bd.inp(f"g2{l}", [128, D]), wfi=bd.inp(f"wfi{l}", [D, 2 * FF]), wfo=bd.inp(f"wfo{l}", [FF, D])))
    out_d = bd.out("out", [TS, D])
    x1_d = bd.scratch("x1", [TS, D])
    xmid_d = [bd.scratch(f"xmid{l}", [TS, D]) for l in range(2)]
    hT_loc = [[bd.dram(f"hTloc{l}_{c}", [D, 512], BF16) for c in range(NCH)] for l in range(2)]
    gatH = [[bd.dram(f"gatH{l}_{c}", [4 * D, 512], BF16) for c in range(NCH)] for l in range(2)]
    YT_loc = [[bd.dram(f"YTloc{l}_{c}", [192, CW], BF16) for c in range(NYC)] for l in range(2)]
    gatY_full = [bd.dram(f"gatY{l}", [NYC * 768, CW], BF16) for l in range(2)]
    gatY = [[gatY_full[l][c * 768:(c + 1) * 768, :] for c in range(NYC)] for l in range(2)]
    ymine = [bd.dram(f"ymine{l}", [CPS * 768, CW], BF16) for l in range(2)]
    cst = load_consts(bd, k, c_d, full=True)
    jr = nc.partition_id() % 4

    emit_pre(bd, k, cst, x_d, gm_d[0], hT_loc[0], gatH[0], TS)
    for l in range(2):
        last = (l == 1)

        def hT_src(t0, l=l):
            r, off = t0 // TS, t0 % TS
            return gatH[l][off // 512].re("(r c p) t -> p r c t", r=4, p=128)[:, r, :, :]

        emit_mix(bd, k, cst, S, hT_src, L[l]["wh"], L[l]["pm"], L[l]["wl"], YT_loc[l], gatY[l], CW)

        ym = ymine[l]
        gsel = gatY_full[l].ap.rearrange("(j r) t -> j r t", j=4)[bass.ds(jr, 1)]
        k.dma(ym, V(gsel.rearrange("o r t -> (o r) t"), gatY_full[l].buf))
        ymv = ym.re("(x q) t -> q x t", q=64)

        def YT_src(b, h, ymv=ymv):
            x0 = (((b * 512) // CW) * 4 + h) * 3
            col = (b * 512) % CW
            return ymv[:, x0:x0 + 3, col:col + 512]

        emit_tail(bd, k, cst, TS, x_d if l == 0 else x1_d, hT_loc[l], YT_src, L[l]["wg"], L[l]["wout"], L[l]["wo"],
                  L[l]["g2"], L[l]["wfi"], L[l]["wfo"], gn_d[l], xmid_d[l], out_d if last else x1_d,
                  None if last else hT_loc[1], None if last else gatH[1], last)
    return bd.finish()


def mix_cols(h):
    z = [-1] * 64
    r = lambda a: list(range(a, a + 64))
    cols = (r(h * 64) + r(256 + h * 64) + r(512 + h * 64) + r(768 + h * 64) + r(1280 + h * 64)
            + [1540 + h] * 64 + [1536 + h] * 64 + r(1544 + h * 64) + z + r(1800 + h * 64) + z
            + r(1024 + h * 64) + r(2056 + h * 64) + z)
    assert len(cols) == WH_COLS
    return np.array(cols)


def gather_cols(w, cols):
    out = np.zeros((w.shape[0], len(cols)), np.float32)
    m = cols >= 0
    out[:, m] = w[:, cols[m]]
    return out


def rep128(g):
    return np.ascontiguousarray(np.broadcast_to(np.asarray(g, np.float32)[None, :], (128, g.shape[0])))


def mix_params(inp, l, h):
    sl = slice(h * 64, (h + 1) * 64)
    pm = np.zeros((64, 17), np.float32)
    pm[:, 0:4] = inp["conv_lru_w"][l][:, sl].T
    pm[:, 4:8] = inp["conv_mlstm_w"][l][:, sl].T
    pm[:, 8:12] = inp["conv_mlstm_w"][l][:, 256 + h * 64:256 + (h + 1) * 64].T
    pm[:, 12] = inp["lru_ba"][l][sl]
    pm[:, 13] = inp["lru_bx"][l][sl]
    pm[:, 14] = inp["lru_lambda"][l][sl]
    pm[:, 15] = inp["mlstm_ig_b"][l][h]
    pm[:, 16] = inp["mlstm_fg_b"][l][h]
    wl = np.concatenate([inp["lru_wa"][l][h], inp["lru_wx"][l][h]], axis=1).astype(np.float32)
    return pm, np.ascontiguousarray(wl)


_CACHE = {}


def kernel(**inputs):
    inp = {k_: np.asarray(v) for k_, v in inputs.items()}
    x = np.asarray(inp["x"], np.float32)
    B, S, _ = x.shape
    TS = S // 4
    cores = list(range(NCORES))
    if S not in _CACHE:
        _CACHE[S] = build_fused(S)
    nc = _CACHE[S]
    cst = const_table()
    shared = {"cst": cst, "gmix0": rep128(inp["norm_mix_g"][0]), "gn0": rep128(inp["norm_mix_g"][1]),
              "gn1": rep128(inp["final_norm_g"])}
    for l in range(2):
        w_in = np.asarray(inp["w_in"][l], np.float32)
        wout = np.concatenate([inp["w_out_lru"][l], inp["w_out_mlstm"][l], inp["w_out_sb"][l]], axis=0)
        shared[f"wg{l}"] = np.ascontiguousarray(w_in[:, 2312:5384])
        shared[f"wout{l}"] = np.ascontiguousarray(np.asarray(wout, np.float32))
        shared[f"wo{l}"] = np.asarray(inp["w_o"][l], np.float32)
        shared[f"g2{l}"] = rep128(inp["norm_ffn_g"][l])
        shared[f"wfi{l}"] = np.asarray(inp["w_ffn_in"][l], np.float32)
        shared[f"wfo{l}"] = np.asarray(inp["w_ffn_out"][l], np.float32)
    ims = []
    for c in cores:
        b, j = c // 4, c % 4
        m = dict(shared)
        m["x"] = np.ascontiguousarray(x[b, j * TS:(j + 1) * TS])
        for l in range(2):
            pm, wl = mix_params(inp, l, j)
            m[f"wh{l}"] = gather_cols(np.asarray(inp["w_in"][l], np.float32), mix_cols(j))
            m[f"pm{l}"] = pm
            m[f"wl{l}"] = wl
        ims.append(m)
    res = run_bass_kernel_spmd(nc, ims, core_ids=cores)
    out = np.zeros((B, S, D), np.float32)
    for c in cores:
        out[c // 4, (c % 4) * TS:(c % 4 + 1) * TS] = np.asarray(res.results[c]["out"])
    return out
```

```python
import math
import numpy as np
import ml_dtypes
import concourse.bass as bass
import concourse.mybir as mybir
from concourse.alu_op_type import AluOpType as ALU
from concourse.bass_utils import run_bass_kernel_spmd

F32 = mybir.dt.float32
BF16 = mybir.dt.bfloat16
AF = mybir.ActivationFunctionType

D = 1024
FF = 2816
NCORES = 8
EPS = 1e-6
GELU_C = math.sqrt(2.0 / math.pi)

PE, ACT, DVE, POOL, SP = "pe", "act", "dve", "pool", "sp"
ENGS = (PE, ACT, DVE, POOL, SP)
N_DMA_SEMS = 10
N_CC_SEMS = 4


class Buf:
    __slots__ = ("name", "w", "r")

    def __init__(self, name):
        self.name = name
        self.w = None
        self.r = {}


class V:
    __slots__ = ("ap", "buf")

    def __init__(self, ap, buf):
        self.ap = ap
        self.buf = buf

    def __getitem__(self, k):
        return V(self.ap[k], self.buf)

    def re(self, s, **kw):
        return V(self.ap.rearrange(s, **kw), self.buf)

    def bc(self, dt):
        return V(self.ap.bitcast(dt), self.buf)


class Prog:
    def __init__(self, nc, same_engine_sync=True):
        self.nc = nc
        self.q = {e: [] for e in ENGS}
        self.sems = {}
        self.cnt = {}
        self.seen = {e: {} for e in ENGS}
        self.same = same_engine_sync
        self.dma_rr = {e: 0 for e in ENGS}
        self.cc_rr = 0
        self.dma_last = {}
        self.epoch = 0
        self._ctx = []

    def _mk(self, key):
        cm = self.nc.semaphore("s_" + "_".join(str(k) for k in key))
        self.sems[key] = cm.__enter__()
        self._ctx.append(cm)
        self.cnt[key] = 0

    def close(self):
        for cm in reversed(self._ctx):
            cm.__exit__(None, None, None)

    def _ckey(self, eng):
        key = ("c", eng, self.epoch)
        if key not in self.sems:
            self._mk(key)
        return key

    def _need(self, eng, waits, key, val):
        if key[0] == "c":
            if key[2] < self.epoch:
                return
            if key[1] == eng and (eng == PE or not self.same):
                return
        if self.seen[eng].get(key, 0) >= val:
            return
        if waits.get(key, 0) < val:
            waits[key] = val

    def op(self, eng, fn, reads=(), writes=(), dma=False, inc=True):
        waits = {}
        for v in reads:
            b = v.buf if isinstance(v, V) else v
            if b is not None and b.w is not None:
                self._need(eng, waits, *b.w)
        for v in writes:
            b = v.buf if isinstance(v, V) else v
            if b is None:
                continue
            if b.w is not None:
                self._need(eng, waits, *b.w)
            for k, val in b.r.items():
                self._need(eng, waits, k, val)
        if dma == "cc":
            i = self.cc_rr
            self.cc_rr = (i + 1) % N_CC_SEMS
            key = ("cc", i)
            step = 1
        elif dma:
            i = self.dma_rr[eng]
            self.dma_rr[eng] = (i + 1) % N_DMA_SEMS
            key = ("d", eng, i)
            step = 16
        else:
            key = self._ckey(eng)
            step = 1
        if dma:
            if key not in self.sems:
                self._mk(key)
            if key in self.dma_last:
                self._need(eng, waits, *self.dma_last[key])
        for k, v in waits.items():
            self.seen[eng][k] = v
        if inc:
            self.cnt[key] += step
            tok = (key, self.cnt[key])
        else:
            tok = (key, self.cnt[key] + step)
        if dma:
            self.dma_last[key] = tok
        self.q[eng].append((list(waits.items()), fn, key, step if inc else 0))
        for v in reads:
            b = v.buf if isinstance(v, V) else v
            if b is not None:
                if b.r.get(key, 0) < tok[1]:
                    b.r[key] = tok[1]
        for v in writes:
            b = v.buf if isinstance(v, V) else v
            if b is not None:
                b.w = tok
                b.r = {}
        return tok

    def barrier(self):
        snap = {k: c for k, c in self.cnt.items() if c > 0 and not (k[0] == "c" and k[2] < self.epoch)}
        for e in ENGS:
            waits = {}
            for k, c in snap.items():
                self._need(e, waits, k, c)
            for k, v in waits.items():
                self.seen[e][k] = v
            if waits:
                self.q[e].append((list(waits.items()), None, None, 0))
        self.epoch += 1

    def emit(self):
        nc = self.nc
        with nc.Block() as block:
            def run(e):
                def body(eng):
                    for waits, fn, key, inc in self.q[e]:
                        for k, v in waits:
                            eng.wait_ge(self.sems[k], v)
                        if fn is not None:
                            ins = fn(eng)
                            if inc:
                                if key[0] == "cc":
                                    ins.then_inc(self.sems[key])
                                else:
                                    ins.then_inc(self.sems[key], inc)
                return body
            block.tensor(run(PE))
            block.scalar(run(ACT))
            block.vector(run(DVE))
            block.gpsimd(run(POOL))
            block.sync(run(SP))


def _ap(x):
    return x.ap if isinstance(x, V) else x


def _vs(*xs):
    return [x for x in xs if isinstance(x, V)]


class K:
    def __init__(self, nc, P, arena, banks):
        self.nc = nc
        self.P = P
        self.arena = arena
        self.off = 0
        self.banks = banks
        self.bank_rr = 0
        self.marks = []

    def alloc(self, name, shape, dt, parts=128):
        n = int(np.prod(shape))
        nb = n * (4 if dt == F32 else 2)
        nb = (nb + 31) // 32 * 32
        ap = self.arena[:, self.off // 2:(self.off + nb) // 2]
        self.off += nb
        assert self.off <= 206 * 1024, (name, self.off)
        if dt == F32:
            ap = ap.bitcast(F32)
        ap = ap[:, 0:n]
        if parts != 128:
            ap = ap[0:parts]
        if len(shape) == 2:
            ap = ap.rearrange("p (a b) -> p a b", a=shape[0])
        elif len(shape) == 3:
            ap = ap.rearrange("p (a b c) -> p a b c", a=shape[0], b=shape[1])
        return V(ap, Buf(name))

    def mark(self):
        self.marks.append(self.off)

    def release(self):
        self.off = self.marks.pop()

    def bank(self):
        b = self.banks[self.bank_rr]
        self.bank_rr = (self.bank_rr + 1) % len(self.banks)
        return b

    def mm(self, out, lhsT, rhs, start, stop, inc=None, sgc=False):
        o, l, r = _ap(out), _ap(lhsT), _ap(rhs)
        self.P.op(PE, lambda e: e.matmul(o, lhsT=l, rhs=r, start=start, stop=stop, skip_group_check=sgc),
                  reads=_vs(lhsT, rhs), writes=_vs(out), inc=(stop if inc is None else inc))

    def tr(self, out, in_, ident, inc=True):
        o, i, d = _ap(out), _ap(in_), _ap(ident)
        self.P.op(PE, lambda e: e.transpose(o, i, d), reads=_vs(in_, ident), writes=_vs(out), inc=inc)

    def allgather(self, out, in_, groups):
        o, i = _ap(out), _ap(in_)
        self.P.op(POOL, lambda e: e.collective_compute("AllGather", ALU.bypass, replica_groups=groups,
                                                       ins=[i], outs=[o]),
                  reads=_vs(in_), writes=_vs(out), dma="cc")

    def act(self, out, in_, func, bias=None, scale=None, accum=None):
        o, i = _ap(out), _ap(in_)
        kw = {}
        if bias is not None:
            kw["bias"] = _ap(bias)
        if scale is not None:
            kw["scale"] = _ap(scale)
        if accum is not None:
            kw["accum_out"] = _ap(accum)
        self.P.op(ACT, lambda e: e.activation(out=o, in_=i, func=func, **kw),
                  reads=_vs(in_, bias, scale), writes=_vs(out, accum))

    def tt(self, out, in0, in1, op, eng=DVE):
        o, a, b = _ap(out), _ap(in0), _ap(in1)
        self.P.op(eng, lambda e: e.tensor_tensor(out=o, in0=a, in1=b, op=op),
                  reads=_vs(in0, in1), writes=_vs(out))

    def ts(self, out, in0, s1, op0, s2=None, op1=None, eng=DVE):
        o, a, x1, x2 = _ap(out), _ap(in0), _ap(s1), _ap(s2)
        if op1 is None:
            fn = lambda e: e.tensor_scalar(out=o, in0=a, scalar1=x1, scalar2=None, op0=op0)
        else:
            fn = lambda e: e.tensor_scalar(out=o, in0=a, scalar1=x1, scalar2=x2, op0=op0, op1=op1)
        self.P.op(eng, fn, reads=_vs(in0, s1, s2), writes=_vs(out))

    def stt(self, out, in0, scalar, in1, op0, op1, eng=DVE):
        o, a, s, b = _ap(out), _ap(in0), _ap(scalar), _ap(in1)
        self.P.op(eng, lambda e: e.scalar_tensor_tensor(out=o, in0=a, scalar=s, in1=b, op0=op0, op1=op1),
                  reads=_vs(in0, scalar, in1), writes=_vs(out))

    def recip(self, out, in_):
        o, i = _ap(out), _ap(in_)
        self.P.op(DVE, lambda e: e.reciprocal(out=o, in_=i), reads=_vs(in_), writes=_vs(out))

    def copy(self, out, in_, eng=DVE):
        o, i = _ap(out), _ap(in_)
        self.P.op(eng, lambda e: e.tensor_copy(out=o, in_=i), reads=_vs(in_), writes=_vs(out))

    def memset(self, out, val, eng=DVE):
        o = _ap(out)
        self.P.op(eng, lambda e: e.memset(o, val), writes=_vs(out))

    def scan(self, out, d0, d1, initial, op0, op1):
        o, a, b, i = _ap(out), _ap(d0), _ap(d1), _ap(initial)
        self.P.op(DVE, lambda e: e.tensor_tensor_scan(out=o, data0=a, data1=b, initial=i, op0=op0, op1=op1),
                  reads=_vs(d0, d1, initial), writes=_vs(out))

    def dma(self, out, in_, eng=SP):
        o, i = _ap(out), _ap(in_)
        self.P.op(eng, lambda e: e.dma_start(out=o, in_=i), reads=_vs(in_), writes=_vs(out), dma=True)

    def sigmoid_act(self, out, in_, tmp1, tmp2, scale=1.0, bias=None):
        self.act(tmp1, in_, AF.Exp, scale=-scale, bias=bias)
        self.act(tmp2, tmp1, AF.Ln, bias=1.0)
        self.act(out, tmp2, AF.Exp, scale=-1.0)


def load_cast(k, dst, src_ap, ncols, step=2048):
    c = 0
    while c < ncols:
        n = min(step, ncols - c)
        k.dma(dst[:, c:c + n], src_ap[:, c:c + n], eng=POOL)
        c += n


def rms_rstd(k, x, junk, ss, lnv, rstd, eps_col):
    k.act(junk, x, AF.Square, accum=ss)
    k.act(lnv, ss, AF.Ln, scale=1.0 / D, bias=eps_col)
    k.act(rstd, lnv, AF.Exp, scale=-0.5)


def norm_to_hT(k, x, g_bc, dstT, cst, tmp):
    rms_rstd(k, x, tmp["junk"], tmp["ss"], tmp["lnv"], tmp["rstd"], cst["eps"])
    k.stt(tmp["hb"], x, tmp["rstd"], g_bc, ALU.mult, ALU.mult)
    bank = k.bank()
    pT = bank.bc(BF16)
    for c in range(8):
        k.tr(pT[:, c * 128:(c + 1) * 128], tmp["hb"][:, c * 128:(c + 1) * 128], cst["ident"], inc=(c == 7))
    k.act(dstT, pT.re("p (c t) -> p c t", c=8), AF.Copy)


class Builder:
    def __init__(self, name):
        self.nc = bass.Bass("TRN2", target_bir_lowering=False)
        self.stack = []
        nc = self.nc
        self.P = Prog(nc)
        cm = nc.sbuf_tensor("arena", [128, 103 * 1024], BF16)
        arena = cm.__enter__()
        self.stack.append(cm)
        cm = nc.psum_tensor("ps_all", [128, 4096], F32)
        ps_all = cm.__enter__()
        self.stack.append(cm)
        self.ps_all = ps_all
        banks = [V(ps_all[:, i * 512:(i + 1) * 512], Buf(f"ps{i}")) for i in range(8)]
        self.k = K(nc, self.P, arena[:], banks)
        self.k.ps_all = ps_all
        self.outs = []

    def inp(self, name, shape, dt=F32):
        return V(self.nc.dram_tensor(name, list(shape), dt, kind="ExternalInput").ap(), Buf(name))

    def out(self, name, shape, dt=F32):
        v = V(self.nc.dram_tensor(name, list(shape), dt, kind="ExternalOutput").ap(), Buf(name))
        self.outs.append(v)
        return v

    def scratch(self, name, shape, dt=F32):
        return V(self.nc.dram_tensor(name, list(shape), dt, kind="Internal").ap(), Buf(name))

    def dram(self, name, shape, dt=F32):
        return V(self.nc.dram_tensor(name, list(shape), dt).ap(), Buf(name))

    def finish(self):
        P = self.P
        waits = {}
        for v in self.outs:
            if v.buf.w is not None:
                P._need(SP, waits, *v.buf.w)
        P.q[SP].append((list(waits.items()), None, None, 0))
        P.emit()
        P.close()
        for cm in reversed(self.stack):
            cm.__exit__(None, None, None)
        return self.nc


def load_consts(bd, k, cdram):
    cst = {}
    cst["ident"] = k.alloc("ident", (128,), BF16)
    cst["eps"] = k.alloc("eps", (1,), F32)
    k.dma(cst["ident"], cdram[:, 0:128], eng=POOL)
    k.memset(cst["eps"], EPS)
    return cst


def const_table():
    t = np.zeros((128, 128), np.float32)
    t[:, 0:128] = np.eye(128, dtype=np.float32)
    return t


GROUPS = [[0, 1, 2, 3], [4, 5, 6, 7]]


def emit_pre(bd, k, cst, x_d, g_d, hT_loc, gatH, TS):
    k.mark()
    g_bc = k.alloc("g_bc", (1024,), F32)
    k.dma(g_bc, g_d)
    xs = [k.alloc(f"pre_x{i}", (1024,), F32) for i in range(2)]
    stg = [k.alloc(f"pre_stg{i}", (8, 512), BF16) for i in range(2)]
    tmp = dict(junk=V(k.alloc("pre_junk", (1024,), BF16).ap, None),
               ss=k.alloc("pre_ss", (1,), F32), lnv=k.alloc("pre_lnv", (1,), F32),
               rstd=k.alloc("pre_rstd", (1,), F32), hb=k.alloc("pre_hb", (1024,), BF16))
    nt = TS // 128
    k.dma(xs[0], x_d[0:128, :])
    for i in range(nt):
        if i + 1 < nt:
            k.dma(xs[(i + 1) % 2], x_d[(i + 1) * 128:(i + 2) * 128, :])
        sg = stg[(i // 4) % 2]
        norm_to_hT(k, xs[i % 2], g_bc, sg[:, :, (i % 4) * 128:(i % 4 + 1) * 128], cst, tmp)
        if i % 4 == 3:
            c = i // 4
            k.dma(hT_loc[c].re("(c p) t -> p c t", p=128), sg)
            k.allgather(gatH[c], hT_loc[c], GROUPS)
    k.release()
    k.P.barrier()


def emit_tail(bd, k, cst, TS, x_d, hT_loc, YT_src, wg_d, wout_d, wo_d, g2_d, wfi_d, wfo_d, gn_d,
              xmid_d, xout_d, hTn_loc, gatHn, last):
    P = k.P
    k.mark()
    wg = [[k.alloc(f"wg{i}_{p}", (1536,), BF16) for p in range(2)] for i in range(8)]
    wout = [k.alloc(f"wout{i}", (1024,), BF16) for i in range(6)]
    wo = [k.alloc(f"wo{i}", (1024,), BF16) for i in range(8)]
    for kc in range(8):
        k.dma(wg[kc][0], wg_d[kc * 128:(kc + 1) * 128, 0:1536], eng=POOL)
    for kc in range(6):
        load_cast(k, wout[kc], wout_d[kc * 128:(kc + 1) * 128, :], 1024)
    for kc in range(8):
        k.dma(wg[kc][1], wg_d[kc * 128:(kc + 1) * 128, 1536:3072], eng=POOL)
    for kc in range(8):
        load_cast(k, wo[kc], wo_d[kc * 128:(kc + 1) * 128, :], 1024)
    hTb = [k.alloc(f"a_hT{i}", (8, 512), BF16) for i in range(2)]
    YTb = [k.alloc(f"a_YT{i}", (6, 512), BF16) for i in range(2)]
    xb = [k.alloc(f"a_x{i}", (4, 1024), F32) for i in range(2)]
    mT = k.alloc("a_mT", (8, 512), BF16)
    e1 = [k.alloc(f"a_e1{i}", (512,), F32) for i in range(2)]
    e2 = [k.alloc(f"a_e2{i}", (512,), F32) for i in range(2)]
    sg = [k.alloc(f"a_sg{i}", (512,), F32) for i in range(2)]
    macc = k.alloc("a_macc", (512,), F32)
    mtmp = k.alloc("a_mtmp", (512,), F32)
    x_v = x_d.re("(i p) d -> p i d", p=128)
    xmid_v = xmid_d.re("(i p) d -> p i d", p=128)
    nblk = TS // 512

    def loadA(b):
        t0 = b * 512
        k.dma(hTb[b % 2], hT_loc[b].re("(c p) t -> p c t", p=128))
        for h in range(4):
            pb = (h % 2) * 64
            k.dma(YTb[b % 2][pb:pb + 64, (h // 2)::2, :], YT_src(b, h))
        k.dma(xb[b % 2], x_v[:, b * 4:(b + 1) * 4, :])

    loadA(0)
    cnt = 0
    for b in range(nblk):
        if b + 1 < nblk:
            loadA(b + 1)
        hT, YT, x = hTb[b % 2], YTb[b % 2], xb[b % 2]
        for oc in range(8):
            for br in range(3):
                G = k.bank()
                for kc in range(8):
                    c0 = br * 1024 + oc * 128
                    k.mm(G, wg[kc][c0 // 1536][:, c0 % 1536: c0 % 1536 + 128], hT[:, kc, :],
                         kc == 0, kc == 7)
                Y = k.bank()
                for kc in range(2):
                    k.mm(Y, wout[br * 2 + kc][:, oc * 128:(oc + 1) * 128], YT[:, br * 2 + kc, :],
                         kc == 0, kc == 1)
                j = cnt % 2
                cnt += 1
                k.sigmoid_act(sg[j], G, e1[j], e2[j])
                if br == 0:
                    k.tt(macc, Y, sg[j], ALU.mult)
                elif br == 1:
                    k.tt(mtmp, Y, sg[j], ALU.mult)
                    k.tt(macc, macc, mtmp, ALU.add)
                else:
                    k.tt(mtmp, Y, sg[j], ALU.mult)
                    k.tt(mT[:, oc, :], macc, mtmp, ALU.add)
        for i in range(4):
            for ch in range(2):
                O = k.bank()
                for kc in range(8):
                    k.mm(O, mT[:, kc, i * 128:(i + 1) * 128], wo[kc][:, ch * 512:(ch + 1) * 512],
                         kc == 0, kc == 7)
                k.tt(x[:, i, ch * 512:(ch + 1) * 512], x[:, i, ch * 512:(ch + 1) * 512], O, ALU.add)
        k.dma(V(xmid_v.ap[:, b * 4:(b + 1) * 4, :], Buf("xmid_blk")), x)
    k.release()
    P.barrier()

    k.mark()
    TB = 256
    NT = TB // 128
    wfi = [[k.alloc(f"wfi{i}_{p}", (1408,), BF16) for p in range(4)] for i in range(8)]
    wfo = [k.alloc(f"wfo{i}", (1024,), BF16) for i in range(22)]
    g2 = k.alloc("g2", (1024,), F32)
    gn = k.alloc("gn", (1024,), F32)
    k.dma(g2, g2_d)
    k.dma(gn, gn_d)
    for pc in (0, 2, 1, 3):
        for kc in range(8):
            k.dma(wfi[kc][pc], wfi_d[kc * 128:(kc + 1) * 128, pc * 1408:(pc + 1) * 1408], eng=POOL)
    for kc in range(22):
        load_cast(k, wfo[kc], wfo_d[kc * 128:(kc + 1) * 128, :], 1024)
    xm = [k.alloc(f"b_x{i}", (NT, 1024), F32) for i in range(2)]
    hfT = k.alloc("b_hfT", (8, TB), BF16)
    aT = k.alloc("b_aT", (22, TB), BF16)
    e1 = [k.alloc(f"b_e1{i}", (TB,), F32) for i in range(2)]
    e2 = [k.alloc(f"b_e2{i}", (TB,), F32) for i in range(2)]
    sg = [k.alloc(f"b_sg{i}", (TB,), F32) for i in range(2)]
    t1 = [k.alloc(f"b_t1{i}", (TB,), F32) for i in range(2)]
    tmp = dict(junk=V(k.alloc("b_junk", (1024,), BF16).ap, None),
               ss=k.alloc("b_ss", (1,), F32), lnv=k.alloc("b_lnv", (1,), F32),
               rstd=k.alloc("b_rstd", (1,), F32), hb=k.alloc("b_hb", (1024,), BF16))
    stg = k.alloc("b_stg", (8, TB), BF16)
    of = k.alloc("b_of", (NT, 1024), F32)
    nblk = TS // TB
    xout_v = xout_d.re("(i p) d -> p i d", p=128)

    def loadB(b):
        k.dma(xm[b % 2], xmid_v[:, b * NT:(b + 1) * NT, :])

    loadB(0)
    cnt = 0
    for b in range(nblk):
        if b + 1 < nblk:
            loadB(b + 1)
        x = xm[b % 2]
        for i in range(NT):
            norm_to_hT(k, x[:, i, :], g2, hfT[:, :, i * 128:(i + 1) * 128], cst, tmp)
        for hc in range(22):
            Gt = k.bank()
            Up = k.bank()
            for kc in range(8):
                k.mm(Gt[:, 0:TB], wfi[kc][(hc * 128) // 1408][:, (hc * 128) % 1408:(hc * 128) % 1408 + 128],
                     hfT[:, kc, :], kc == 0, kc == 7)
            for kc in range(8):
                u0 = FF + hc * 128
                k.mm(Up[:, 0:TB], wfi[kc][u0 // 1408][:, u0 % 1408:u0 % 1408 + 128], hfT[:, kc, :],
                     kc == 0, kc == 7)
            j = cnt % 2
            cnt += 1
            k.sigmoid_act(sg[j], Gt[:, 0:TB], e1[j], e2[j])
            k.tt(t1[j], Gt[:, 0:TB], sg[j], ALU.mult)
            k.tt(aT[:, hc, :], t1[j], Up[:, 0:TB], ALU.mult)
        for i in range(NT):
            for ch in range(2):
                O = k.bank()
                for hc in range(22):
                    k.mm(O, aT[:, hc, i * 128:(i + 1) * 128], wfo[hc][:, ch * 512:(ch + 1) * 512],
                         hc == 0, hc == 21)
                k.tt(x[:, i, ch * 512:(ch + 1) * 512], x[:, i, ch * 512:(ch + 1) * 512], O, ALU.add)
        if not last:
            k.dma(V(xout_v.ap[:, b * NT:(b + 1) * NT, :], Buf("xout_blk")), x)
            for i in range(NT):
                norm_to_hT(k, x[:, i, :], gn, stg[:, :, i * 128:(i + 1) * 128], cst, tmp)
            c = (b * TB) // 512
            k.dma(hTn_loc[c].re("(c p) t -> p c t", p=128)[:, :, (b * TB) % 512:(b * TB) % 512 + TB], stg)
            if (b * TB) % 512 + TB == 512:
                k.allgather(gatHn[c], hTn_loc[c], GROUPS)
        else:
            for i in range(NT):
                rms_rstd(k, x[:, i, :], tmp["junk"], tmp["ss"], tmp["lnv"], tmp["rstd"], cst["eps"])
                k.stt(of[:, i, :], x[:, i, :], tmp["rstd"], gn, ALU.mult, ALU.mult)
            k.dma(V(xout_v.ap[:, b * NT:(b + 1) * NT, :], Buf("xout_blk")), of)
    k.release()
    P.barrier()


C_LX, C_LG, C_MQ, C_MK, C_MO, C_F, C_I, C_SQ, C_SK, C_MV, C_SV, WH_COLS = \
    0, 64, 128, 192, 256, 320, 384, 448, 576, 704, 768, 896
CT_ID, CT_NTRI, CT_MM, CT_RST, CT_ATT, CT_COLS = 0, 128, 256, 384, 896, 2944


def const_table():
    t = np.zeros((128, CT_COLS), np.float32)
    t[:, CT_ID:CT_ID + 128] = np.eye(128, dtype=np.float32)
    j = np.arange(128)[:, None]
    s = np.arange(128)[None, :]
    t[:, CT_NTRI:CT_NTRI + 128] = -(j >= s).astype(np.float32)
    t[:, CT_MM:CT_MM + 128] = (j <= s).astype(np.float32)
    r = np.ones(512, np.float32)
    r[::128] = 0.0
    t[:, CT_RST:CT_RST + 512] = r[None, :]
    tq = np.arange(512)[None, :]
    for kj in range(4):
        t[:, CT_ATT + kj * 512: CT_ATT + (kj + 1) * 512] = ((kj * 128 + j) < tq).astype(np.float32)
    return t


def load_consts(bd, k, cdram, full=False):
    cst = {}
    n = CT_COLS if full else 128
    cb = k.alloc("cb", (n,), BF16)
    load_cast(k, cb, cdram, n)
    cst["ident"] = cb[:, CT_ID:CT_ID + 128]
    if full:
        cst["ntri"] = cb[:, CT_NTRI:CT_NTRI + 128]
        cst["mmask"] = cb[:, CT_MM:CT_MM + 128]
        cst["rst"] = cb[0:64, CT_RST:CT_RST + 512]
        cst["att"] = cb[:, CT_ATT:CT_ATT + 2048].re("p (j t) -> p j t", j=4)
        cst["nones"] = k.alloc("nones", (128,), BF16)
        k.memset(cst["nones"], -1.0)
    cst["eps"] = k.alloc("eps", (1,), F32)
    k.memset(cst["eps"], EPS)
    return cst


def conv4(k, out, xin, cw):
    k.ts(out, xin[:, 0:512], cw[:, 0:1], ALU.mult)
    for j in range(1, 4):
        k.stt(out, xin[:, j:j + 512], cw[:, j:j + 1], out, ALU.mult, ALU.add)
    k.copy(xin[:, 0:3], xin[:, 512:515])


def emit_mix(bd, k, cst, S, hT_src, wh_d, pm_d, wl_d, YT_loc, gatY, CW):
    def YT_dst(r0, t0):
        return YT_loc[t0 // CW][r0:r0 + 64, t0 % CW:t0 % CW + 512]

    P = k.P
    k.mark()
    NB = S // 512
    allb = k.banks
    k.banks = allb[0:6]
    k.bank_rr = 0
    bnum, bden = allb[6], allb[7]

    qT = k.alloc("qT", (S,), BF16)
    kT = k.alloc("kT", (S,), BF16)
    vsb = k.alloc("vsb", (S // 128, 128), BF16)
    W = [k.alloc(f"W{i}", (WH_COLS,), BF16) for i in range(8)]
    for kc in range(8):
        load_cast(k, W[kc], wh_d[kc * 128:(kc + 1) * 128, :], WH_COLS)
    pm = k.alloc("pm", (24,), F32, parts=64)
    k.dma(pm[:, 0:17], pm_d)
    wl = k.alloc("wl", (128,), BF16, parts=64)
    load_cast(k, wl, wl_d, 128)
    k.mark()

    def T(name, n=512, dt=F32, parts=64):
        return k.alloc(name, (n,), dt, parts)

    nba, nbx, nfb, ibm, nspl8 = pm[:, 17:18], pm[:, 18:19], pm[:, 19:20], pm[:, 20:21], pm[:, 21:22]
    k.ts(nba, pm[:, 12:13], -1.0, ALU.mult)
    k.ts(nbx, pm[:, 13:14], -1.0, ALU.mult)
    k.ts(nfb, pm[:, 16:17], -1.0, ALU.mult)
    k.ts(ibm, pm[:, 15:16], -math.log(8.0), ALU.add)
    k.act(pm[:, 22:23], pm[:, 14:15], AF.Exp, scale=-1.0)
    k.act(pm[:, 23:24], pm[:, 22:23], AF.Ln, bias=1.0)
    k.ts(nspl8, pm[:, 23:24], -8.0, ALU.mult)

    hTb = [k.alloc(f"m_hT{i}", (8, 512), BF16) for i in range(2)]
    xl, xq, xk = T("xl", 515), T("xq", 515), T("xk", 515)
    for t in (xl, xq, xk):
        k.memset(t[:, 0:3], 0.0)
    gen = [T(f"gen{i}") for i in range(16)]
    xabf = T("xabf", dt=BF16)
    tA, tB = T("tA"), T("tB")
    xa, gx, r_, ig, a_, a2, m1, u_, x2, p_, inner, sgl, yat = gen[0:13]
    cq, ck, ef, lf, nb, eb, itmp, ek, sq_, sk_, q1, k1, so, dd, rd, hm = gen[0:16]
    hl = [T("hl0"), T("hl1")]
    ya_o = [T("ya_o0", dt=BF16), T("ya_o1", dt=BF16)]
    yb_o = [T("yb_o0", dt=BF16), T("yb_o1", dt=BF16)]
    qtl, ktl = T("qtl", dt=BF16), T("ktl", dt=BF16)
    vmb = [k.alloc(f"vmb{i}", (4, 128), BF16) for i in range(2)]
    for v in vmb:
        k.memset(v[:, :, 64:128], 1.0)
    ktok = [k.alloc(f"ktok{i}", (64,), BF16) for i in range(2)]
    PTs = [k.alloc(f"PTs{i}", (128,), BF16) for i in range(2)]
    Wc = [T("Wc0", 128), T("Wc1", 128)]
    Dst = [T("Dst0", 128), T("Dst1", 128)]
    Dbf = [T("Dbf0", 128, BF16), T("Dbf1", 128, BF16)]
    ident = cst["ident"]

    def loadH(b):
        k.dma(hTb[b % 2], hT_src(b * 512))

    loadH(0)
    for b in range(NB):
        if b + 1 < NB:
            loadH(b + 1)
        hT = hTb[b % 2]
        t0 = b * 512
        tsl = slice(t0, t0 + 512)

        def proj(c0, M):
            bk = k.bank()
            for kc in range(8):
                k.mm(bk[0:M, :], W[kc][:, c0:c0 + M], hT[:, kc, :], kc == 0, kc == 7)
            return bk[0:M, :]

        k.act(qT[:, tsl], proj(C_SQ, 128), AF.Copy, scale=0.125)
        k.copy(kT[:, tsl], proj(C_SK, 128))
        bv = k.bank()
        for i in range(4):
            for kc in range(8):
                k.mm(bv[:, i * 128:(i + 1) * 128], hT[:, kc, i * 128:(i + 1) * 128],
                     W[kc][:, C_SV:C_SV + 128], kc == 0, kc == 7)
        k.act(vsb[:, 4 * b:4 * b + 4, :], bv.re("p (i c) -> p i c", i=4), AF.Copy)

        k.act(xl[:, 3:515], proj(C_LX, 64), AF.Copy)
        conv4(k, xa, xl, pm[:, 0:4])
        k.act(xabf, xa, AF.Copy)
        k.act(gx, proj(C_LG, 64), AF.Copy)
        br_ = k.bank()
        k.mm(br_[0:64, :], wl[:, 0:64], xabf, True, True)
        k.sigmoid_act(r_, br_[0:64, :], tA, tB, bias=nba)
        bi_ = k.bank()
        k.mm(bi_[0:64, :], wl[:, 64:128], xabf, True, True)
        k.sigmoid_act(ig, bi_[0:64, :], tA, tB, bias=nbx)
        k.act(a_, r_, AF.Exp, scale=nspl8)
        k.act(a2, a_, AF.Square)
        k.ts(a2, a2, 0.99999994, ALU.min)
        k.act(m1, a2, AF.Ln, scale=-1.0, bias=1.0)
        k.act(m1, m1, AF.Exp, scale=0.5)
        k.tt(u_, ig, xa, ALU.mult)
        k.tt(u_, u_, m1, ALU.mult)
        init = 0.0 if b == 0 else hl[(b - 1) % 2][:, 511:512]
        k.scan(hl[b % 2], a_, u_, init, ALU.mult, ALU.add)
        k.act(x2, gx, AF.Square)
        k.ts(p_, x2, 0.044715, ALU.mult, 1.0, ALU.add)
        k.tt(inner, p_, gx, ALU.mult)
        k.sigmoid_act(sgl, inner, tA, tB, scale=2.0 * GELU_C)
        k.tt(yat, hl[b % 2], gx, ALU.mult)
        k.tt(ya_o[b % 2], yat, sgl, ALU.mult)
        k.dma(YT_dst(0, t0), ya_o[b % 2])

        k.act(xq[:, 3:515], proj(C_MQ, 64), AF.Copy)
        k.act(xk[:, 3:515], proj(C_MK, 64), AF.Copy)
        conv4(k, cq, xq, pm[:, 4:8])
        conv4(k, ck, xk, pm[:, 8:12])
        k.act(ef, proj(C_F, 64), AF.Exp, scale=-1.0, bias=nfb)
        k.act(lf, ef, AF.Ln, bias=1.0)
        k.scan(nb, cst["rst"], lf, 0.0, ALU.mult, ALU.add)
        k.act(eb, nb, AF.Exp, scale=-1.0)
        k.tt(itmp, proj(C_I, 64), nb, ALU.add)
        k.act(ek, itmp, AF.Exp, bias=ibm)
        k.sigmoid_act(sq_, cq, tA, tB)
        k.tt(q1, cq, sq_, ALU.mult)
        k.tt(qtl, q1, eb, ALU.mult)
        k.sigmoid_act(sk_, ck, tA, tB)
        k.tt(k1, ck, sk_, ALU.mult)
        k.tt(ktl, k1, ek, ALU.mult)
        bmv = k.bank()
        for i in range(4):
            for kc in range(8):
                k.mm(bmv[:, i * 64:(i + 1) * 64], hT[:, kc, i * 128:(i + 1) * 128],
                     W[kc][:, C_MV:C_MV + 64], kc == 0, kc == 7)
        vm = vmb[b % 2]
        k.act(vm[:, :, 0:64], bmv[:, 0:256].re("p (i c) -> p i c", i=4), AF.Copy)
        k.sigmoid_act(so, proj(C_MO, 64), tA, tB)
        for c in range(4):
            cs = slice(c * 128, (c + 1) * 128)
            g = 4 * b + c
            bt = k.bank().bc(BF16)
            k.tr(bt[:, 0:64], ktl[:, cs], ident[0:64, 0:64])
            k.copy(ktok[g % 2], bt[:, 0:64])
            bp = k.bank()
            k.mm(bp[:, 0:128], ktl[:, cs], qtl[:, cs], True, True)
            k.tt(PTs[g % 2], bp[:, 0:128], cst["mmask"], ALU.mult)
            Dc = Dbf[g % 2]
            k.mm(bnum[0:64, cs], vm[:, c, 0:64], PTs[g % 2], True, g == 0)
            if g > 0:
                k.mm(bnum[0:64, cs], Dc[:, 0:64], qtl[:, cs], False, True)
            k.mm(bden[0:64, cs], vm[:, c, 64:128], PTs[g % 2], True, g == 0)
            if g > 0:
                k.mm(bden[0:64, cs], Dc[:, 64:128], qtl[:, cs], False, True)
            bu = k.bank()
            k.mm(bu[0:64, 0:128], ktok[g % 2], vm[:, c, :], True, True)
            a_c = eb[:, c * 128 + 127:c * 128 + 128]
            k.act(Wc[g % 2], bu[0:64, 0:128], AF.Identity, scale=a_c)
            if g == 0:
                k.copy(Dst[1], Wc[0])
            else:
                k.stt(Dst[(g + 1) % 2], Dst[g % 2], a_c, Wc[g % 2], ALU.mult, ALU.add)
            k.act(Dbf[(g + 1) % 2], Dst[(g + 1) % 2], AF.Copy)
        k.act(dd, bden[0:64, :], AF.Abs)
        k.ts(dd, dd, 1.0, ALU.max)
        k.recip(rd, dd)
        k.tt(hm, bnum[0:64, :], rd, ALU.mult)
        k.tt(yb_o[b % 2], hm, so, ALU.mult)
        k.dma(YT_dst(64, t0), yb_o[b % 2])
    k.release()
    P.barrier()

    k.mark()
    zp = [V(k.ps_all[:, i * 1024:(i + 1) * 1024], Buf(f"zp{i}")) for i in range(3)]
    ob = allb[6:8]
    e_t = [k.alloc(f"s2e{i}", (1024,), F32) for i in range(2)]
    sp = [k.alloc(f"s2sp{i}", (1024,), BF16) for i in range(3)]
    At = [k.alloc(f"s2A{i}", (1024,), BF16) for i in range(3)]
    accA = [k.alloc(f"s2accA{i}", (512,), BF16) for i in range(2)]
    accM = [k.alloc(f"s2accM{i}", (512,), BF16) for i in range(2)]
    yc_o = [k.alloc(f"s2yc{i}", (512,), BF16, parts=64) for i in range(2)]
    ntri, nones, att = cst["ntri"], cst["nones"], cst["att"]
    H0, H1 = slice(0, 512), slice(512, 1024)
    p_glob = 0
    for qi in range(S // 512):
        q0 = qi * 512
        O = ob[qi % 2]
        kbs = list(range(4 * qi + 3, -1, -1))
        NP = len(kbs) // 2
        qv = qT[:, q0:q0 + 512]

        def stageA(p):
            khi, klo = kbs[2 * p], kbs[2 * p + 1]
            Z = zp[(p_glob + p) % 3]
            k.mm(Z[:, H0], kT[:, khi * 128:(khi + 1) * 128], qv, True, True, inc=False)
            k.mm(Z[:, H1], kT[:, klo * 128:(klo + 1) * 128], qv, True, True, inc=True)
            k.act(e_t[p % 2], Z, AF.Exp)
            k.act(sp[p % 3], e_t[p % 2], AF.Ln, bias=1.0)
            if klo >= 4 * qi:
                k.tt(sp[p % 3][:, H0], sp[p % 3][:, H0], att[:, khi - 4 * qi, :], ALU.mult)
                k.tt(sp[p % 3][:, H1], sp[p % 3][:, H1], att[:, klo - 4 * qi, :], ALU.mult)

        def stageB1(p):
            khi, klo = kbs[2 * p], kbs[2 * p + 1]
            Z = zp[(p_glob + p) % 3]
            s_ = sp[p % 3]
            k.mm(Z[:, H0], ntri, s_[:, H0], False, p == 0, sgc=True)
            if p > 0:
                k.mm(Z[:, H0], nones, accA[p % 2], False, True, sgc=True)
                k.tt(accM[p % 2], accA[p % 2], s_[:, H0], ALU.add)
                mid = accM[p % 2]
            else:
                mid = s_[:, H0]
            k.mm(Z[:, H1], ntri, s_[:, H1], False, False, sgc=True)
            k.mm(Z[:, H1], nones, mid, False, True, sgc=True)
            k.act(At[p % 3], Z, AF.Exp)
            if klo >= 4 * qi:
                k.tt(At[p % 3][:, H0], At[p % 3][:, H0], att[:, khi - 4 * qi, :], ALU.mult)
                k.tt(At[p % 3][:, H1], At[p % 3][:, H1], att[:, klo - 4 * qi, :], ALU.mult)
            if p + 1 < NP:
                k.tt(accA[(p + 1) % 2], mid, s_[:, H1], ALU.add)

        def stageB2(p):
            khi, klo = kbs[2 * p], kbs[2 * p + 1]
            k.mm(O, vsb[:, khi, :], At[p % 3][:, H0], p == 0, False)
            k.mm(O, vsb[:, klo, :], At[p % 3][:, H1], False, p == NP - 1)

        for step in range(NP + 2):
            if step < NP:
                stageA(step)
            if 1 <= step <= NP:
                stageB1(step - 1)
            if step >= 2:
                stageB2(step - 2)
        p_glob += NP
        k.act(yc_o[qi % 2], O[0:64, :], AF.Copy)
        k.dma(YT_dst(128, q0), yc_o[qi % 2])
        if (q0 + 512) % CW == 0:
            c = q0 // CW
            k.allgather(gatY[c], YT_loc[c], GROUPS)
    k.release()
    k.release()
    k.banks = allb
    P.barrier()


def build_fused(S):
    TS = S // 4
    NCH = TS // 512
    CW = min(2048, TS)
    NYC = S // CW
    CPS = TS // CW
    bd = Builder("fused")
    k = bd.k
    nc = bd.nc
    x_d = bd.inp("x", [TS, D])
    c_d = bd.inp("cst", [128, CT_COLS])
    gm_d = [bd.inp("gmix0", [128, D]), bd.inp("gn0", [128, D])]
    gn_d = [gm_d[1], bd.inp("gn1", [128, D])]
    L = []
    for l in range(2):
        L.append(dict(wh=bd.inp(f"wh{l}", [D, WH_COLS]), pm=bd.inp(f"pm{l}", [64, 17]), wl=bd.inp(f"wl{l}", [64, 128]),
                      wg=bd.inp(f"wg{l}", [D, 3072]), wout=bd.inp(f"wout{l}", [768, D]), wo=bd.inp(f"wo{l}", [D, D]),
                      g2=bd.inp(f"g2{l}", [128, D]), wfi=bd.inp(f"wfi{l}", [D, 2 * FF]), wfo=bd.inp(f"wfo{l}", [FF, D])))
    out_d = bd.out("out", [TS, D])
    x1_d = bd.scratch("x1", [TS, D])
    xmid_d = [bd.scratch(f"xmid{l}", [TS, D]) for l in range(2)]
    hT_loc = [[bd.dram(f"hTloc{l}_{c}", [D, 512], BF16) for c in range(NCH)] for l in range(2)]
    gatH = [[bd.dram(f"gatH{l}_{c}", [4 * D, 512], BF16) for c in range(NCH)] for l in range(2)]
    YT_loc = [[bd.dram(f"YTloc{l}_{c}", [192, CW], BF16) for c in range(NYC)] for l in range(2)]
    gatY_full = [bd.dram(f"gatY{l}", [NYC * 768, CW], BF16) for l in range(2)]
    gatY = [[gatY_full[l][c * 768:(c + 1) * 768, :] for c in range(NYC)] for l in range(2)]
    ymine = [bd.dram(f"ymine{l}", [CPS * 768, CW], BF16) for l in range(2)]
    cst = load_consts(bd, k, c_d, full=True)
    jr = nc.partition_id() % 4

    emit_pre(bd, k, cst, x_d, gm_d[0], hT_loc[0], gatH[0], TS)
    for l in range(2):
        last = (l == 1)

        def hT_src(t0, l=l):
            r, off = t0 // TS, t0 % TS
            return gatH[l][off // 512].re("(r c p) t -> p r c t", r=4, p=128)[:, r, :, :]

        emit_mix(bd, k, cst, S, hT_src, L[l]["wh"], L[l]["pm"], L[l]["wl"], YT_loc[l], gatY[l], CW)

        ym = ymine[l]
        gsel = gatY_full[l].ap.rearrange("(j r) t -> j r t", j=4)[bass.ds(jr, 1)]
        k.dma(ym, V(gsel.rearrange("o r t -> (o r) t"), gatY_full[l].buf))
        ymv = ym.re("(x q) t -> q x t", q=64)

        def YT_src(b, h, ymv=ymv):
            x0 = (((b * 512) // CW) * 4 + h) * 3
            col = (b * 512) % CW
            return ymv[:, x0:x0 + 3, col:col + 512]

        emit_tail(bd, k, cst, TS, x_d if l == 0 else x1_d, hT_loc[l], YT_src, L[l]["wg"], L[l]["wout"], L[l]["wo"],
                  L[l]["g2"], L[l]["wfi"], L[l]["wfo"], gn_d[l], xmid_d[l], out_d if last else x1_d,
                  None if last else hT_loc[1], None if last else gatH[1], last)
    return bd.finish()


def mix_cols(h):
    z = [-1] * 64
    r = lambda a: list(range(a, a + 64))
    cols = (r(h * 64) + r(256 + h * 64) + r(512 + h * 64) + r(768 + h * 64) + r(1280 + h * 64)
            + [1540 + h] * 64 + [1536 + h] * 64 + r(1544 + h * 64) + z + r(1800 + h * 64) + z
            + r(1024 + h * 64) + r(2056 + h * 64) + z)
    assert len(cols) == WH_COLS
    return np.array(cols)


def gather_cols(w, cols):
    out = np.zeros((w.shape[0], len(cols)), np.float32)
    m = cols >= 0
    out[:, m] = w[:, cols[m]]
    return out


def rep128(g):
    return np.ascontiguousarray(np.broadcast_to(np.asarray(g, np.float32)[None, :], (128, g.shape[0])))


def mix_params(inp, l, h):
    sl = slice(h * 64, (h + 1) * 64)
    pm = np.zeros((64, 17), np.float32)
    pm[:, 0:4] = inp["conv_lru_w"][l][:, sl].T
    pm[:, 4:8] = inp["conv_mlstm_w"][l][:, sl].T
    pm[:, 8:12] = inp["conv_mlstm_w"][l][:, 256 + h * 64:256 + (h + 1) * 64].T
    pm[:, 12] = inp["lru_ba"][l][sl]
    pm[:, 13] = inp["lru_bx"][l][sl]
    pm[:, 14] = inp["lru_lambda"][l][sl]
    pm[:, 15] = inp["mlstm_ig_b"][l][h]
    pm[:, 16] = inp["mlstm_fg_b"][l][h]
    wl = np.concatenate([inp["lru_wa"][l][h], inp["lru_wx"][l][h]], axis=1).astype(np.float32)
    return pm, np.ascontiguousarray(wl)


_CACHE = {}


def kernel(**inputs):
    inp = {k_: np.asarray(v) for k_, v in inputs.items()}
    x = np.asarray(inp["x"], np.float32)
    B, S, _ = x.shape
    TS = S // 4
    cores = list(range(NCORES))
    if S not in _CACHE:
        _CACHE[S] = build_fused(S)
    nc = _CACHE[S]
    cst = const_table()
    shared = {"cst": cst, "gmix0": rep128(inp["norm_mix_g"][0]), "gn0": rep128(inp["norm_mix_g"][1]),
              "gn1": rep128(inp["final_norm_g"])}
    for l in range(2):
        w_in = np.asarray(inp["w_in"][l], np.float32)
        wout = np.concatenate([inp["w_out_lru"][l], inp["w_out_mlstm"][l], inp["w_out_sb"][l]], axis=0)
        shared[f"wg{l}"] = np.ascontiguousarray(w_in[:, 2312:5384])
        shared[f"wout{l}"] = np.ascontiguousarray(np.asarray(wout, np.float32))
        shared[f"wo{l}"] = np.asarray(inp["w_o"][l], np.float32)
        shared[f"g2{l}"] = rep128(inp["norm_ffn_g"][l])
        shared[f"wfi{l}"] = np.asarray(inp["w_ffn_in"][l], np.float32)
        shared[f"wfo{l}"] = np.asarray(inp["w_ffn_out"][l], np.float32)
    ims = []
    for c in cores:
        b, j = c // 4, c % 4
        m = dict(shared)
        m["x"] = np.ascontiguousarray(x[b, j * TS:(j + 1) * TS])
        for l in range(2):
            pm, wl = mix_params(inp, l, j)
            m[f"wh{l}"] = gather_cols(np.asarray(inp["w_in"][l], np.float32), mix_cols(j))
            m[f"pm{l}"] = pm
            m[f"wl{l}"] = wl
        ims.append(m)
    res = run_bass_kernel_spmd(nc, ims, core_ids=cores)
    out = np.zeros((B, S, D), np.float32)
    for c in cores:
        out[c // 4, (c % 4) * TS:(c % 4 + 1) * TS] = np.asarray(res.results[c]["out"])
    return out
```
